# Optimizing a Trainium2 kernel written in Bass

```python
import jax, jax.numpy as jnp
from jax import lax
import numpy as np

D_MODEL = 1024
BATCH = 8
SEQ = 4096
DEPTH = 4

N_MIXERS = 2
N_MLSTM = (DEPTH + 1) // 2
N_HGRN = DEPTH // 2
CHUNK = 64
M_HEADS = 8
M_DV = D_MODEL // M_HEADS
M_DQK = M_DV // 2
M_QK = M_HEADS * M_DQK
M_V = M_HEADS * M_DV
M_PROJ = 2 * M_QK + 2 * M_V + 2 * M_HEADS
CONV_K = 4
IGATE_CAP = 15.0
HG_EXPAND = 128
HG_HEADS = D_MODEL // HG_EXPAND
HG_DK = HG_EXPAND
HG_DV = D_MODEL // HG_HEADS
HG_PROJ = 2 * HG_HEADS * HG_DK + 2 * D_MODEL
D_FF = 2816
N_EXPERTS = 8
TOP_K = 2
D_FF_EXPERT = D_FF // 2
EPS = 1e-6
NEG = -1e30
F_FLOOR = 1e-30

kernel_name = 'hybrid_mlstm_hgrn2_adaln_moe'


def rmsnorm(x, w):
    x32 = x.astype(jnp.float32)
    y = x32 * lax.rsqrt(jnp.mean(x32 * x32, axis=-1, keepdims=True) + EPS)
    return (y * w.astype(jnp.float32)).astype(x.dtype)


def head_rmsnorm(h, w):
    nh, dh = h.shape[1], h.shape[3]
    y = h * lax.rsqrt(jnp.mean(h * h, axis=-1, keepdims=True) + EPS)
    return y * w.astype(jnp.float32).reshape(1, nh, 1, dh)


def split_heads(a, nh):
    b, s, _ = a.shape
    return a.reshape(b, s, nh, -1).transpose(0, 2, 1, 3)


def merge_heads(a):
    b, h, s, d = a.shape
    return a.transpose(0, 2, 1, 3).reshape(b, s, h * d)


def to_chunks(a):
    b, h, s = a.shape[:3]
    return jnp.moveaxis(a.reshape((b, h, s // CHUNK, CHUNK) + a.shape[3:]), 2, 0)


def from_chunks(a):
    a = jnp.moveaxis(a, 0, 2)
    b, h, nc, l = a.shape[:4]
    return a.reshape((b, h, nc * l) + a.shape[4:])


def causal_conv(x, w, b):
    ch = x.shape[-1]
    y = lax.conv_general_dilated(x, w[:, None, :].astype(x.dtype), window_strides=(1,),
                                 padding=[(w.shape[0] - 1, 0)],
                                 dimension_numbers=('NWC', 'WIO', 'NWC'),
                                 feature_group_count=ch)
    return y + b.astype(x.dtype)


def mlstm_chunkwise(q, k, v, li, f_pre):
    bsz, nh, _, dk = q.shape
    dv = v.shape[-1]
    lf = jax.nn.log_sigmoid(f_pre)
    mask = jnp.tril(jnp.ones((CHUNK, CHUNK), dtype=bool))

    def step(carry, xs):
        c_st, n_st, m_st = carry
        qc, kc, vc, lic, lfc = xs
        b = jnp.cumsum(lfc, axis=-1)
        d = jnp.where(mask, b[..., :, None] - b[..., None, :] + lic[..., None, :], NEG)
        inter = b + m_st[..., None]
        m_t = jnp.maximum(inter, jnp.max(d, axis=-1))
        w_inter = jnp.exp(inter - m_t)
        s = jnp.einsum('bhtd,bhsd->bhts', qc, kc) * jnp.exp(d - m_t[..., None])
        num = (w_inter[..., None] * jnp.einsum('bhtd,bhdv->bhtv', qc, c_st)
               + jnp.einsum('bhts,bhsv->bhtv', s, vc))
        den = w_inter * jnp.einsum('bhtd,bhd->bht', qc, n_st) + jnp.sum(s, axis=-1)
        h = num / jnp.maximum(jnp.abs(den), jnp.exp(-m_t))[..., None]
        b_last = b[..., -1]
        dl = b_last[..., None] - b + lic
        m_new = jnp.maximum(b_last + m_st, jnp.max(dl, axis=-1))
        wk = jnp.exp(dl - m_new[..., None])
        decay = jnp.exp(b_last + m_st - m_new)
        c_new = decay[..., None, None] * c_st + jnp.einsum('bhs,bhsd,bhsv->bhdv', wk, kc, vc)
        n_new = decay[..., None] * n_st + jnp.einsum('bhs,bhsd->bhd', wk, kc)
        return (c_new, n_new, m_new), h

    init = (jnp.zeros((bsz, nh, dk, dv), jnp.float32),
            jnp.zeros((bsz, nh, dk), jnp.float32),
            jnp.zeros((bsz, nh), jnp.float32))
    _, hs = lax.scan(step, init, (to_chunks(q), to_chunks(k), to_chunks(v), to_chunks(li), to_chunks(lf)))
    return from_chunks(hs)


def hgrn2_chunkwise(q, k, log_f, v):
    bsz, nh, _, dk = q.shape
    dv = v.shape[-1]
    mask = jnp.tril(jnp.ones((CHUNK, CHUNK), dtype=bool))

    def step(s_st, xs):
        qc, kc, gc, vc = xs
        bc = jnp.cumsum(gc, axis=2)
        diff = bc[:, :, :, None, :] - bc[:, :, None, :, :]
        decay = jnp.exp(jnp.where(mask[:, :, None], diff, NEG))
        a = jnp.einsum('bhtd,bhsd,bhtsd->bhts', qc, kc, decay)
        o = (jnp.einsum('bhtd,bhdv->bhtv', qc * jnp.exp(bc), s_st)
             + jnp.einsum('bhts,bhsv->bhtv', a, vc))
        b_last = bc[:, :, -1]
        s_new = (jnp.exp(b_last)[..., None] * s_st
                 + jnp.einsum('bhsd,bhsv->bhdv', kc * jnp.exp(b_last[:, :, None] - bc), vc))
        return s_new, o

    init = jnp.zeros((bsz, nh, dk, dv), jnp.float32)
    _, os_ = lax.scan(step, init, (to_chunks(q), to_chunks(k), to_chunks(log_f), to_chunks(v)))
    return from_chunks(os_)


def mlstm_mixer(h, w_in, i_bias, f_bias, conv_w, conv_b, norm_w, w_out):
    proj = h @ w_in
    qk = jax.nn.silu(causal_conv(proj[..., :2 * M_QK], conv_w, conv_b))
    q = split_heads(qk[..., :M_QK], M_HEADS).astype(jnp.float32)
    k = split_heads(qk[..., M_QK:], M_HEADS).astype(jnp.float32) * (M_DQK ** -0.5)
    v = split_heads(proj[..., 2 * M_QK:2 * M_QK + M_V], M_HEADS).astype(jnp.float32)
    o = proj[..., 2 * M_QK + M_V:2 * M_QK + 2 * M_V]
    gates = proj[..., 2 * M_QK + 2 * M_V:].astype(jnp.float32)
    i_pre = gates[..., :M_HEADS] + i_bias.astype(jnp.float32)
    i_pre = IGATE_CAP * jnp.tanh(i_pre / IGATE_CAP)
    f_pre = gates[..., M_HEADS:] + f_bias.astype(jnp.float32)
    hh = mlstm_chunkwise(q, k, v, jnp.swapaxes(i_pre, 1, 2), jnp.swapaxes(f_pre, 1, 2))
    hh = merge_heads(head_rmsnorm(hh, norm_w)).astype(h.dtype) * jax.nn.sigmoid(o)
    return hh @ w_out


def hgrn2_mixer(h, w_in, lb, norm_w, w_out):
    proj = h @ w_in
    fd = HG_HEADS * HG_DK
    q = jax.nn.silu(proj[..., :fd]).astype(jnp.float32)
    f = proj[..., fd:2 * fd].astype(jnp.float32)
    i = proj[..., 2 * fd:2 * fd + D_MODEL].astype(jnp.float32)
    g = proj[..., 2 * fd + D_MODEL:]
    lb = lb.astype(jnp.float32)
    f_t = lb + (1.0 - lb) * jax.nn.sigmoid(f)
    log_f = jnp.log(jnp.maximum(f_t, F_FLOOR))
    k = (1.0 - lb) * jax.nn.sigmoid(-f)
    o = hgrn2_chunkwise(split_heads(q, HG_HEADS), split_heads(k, HG_HEADS),
                        split_heads(log_f, HG_HEADS), split_heads(i, HG_HEADS))
    o = merge_heads(head_rmsnorm(o, norm_w)).astype(h.dtype) * jax.nn.silu(g)
    return o @ w_out


def swiglu(t, wg, wu, wd):
    return (jax.nn.silu(t @ wg) * (t @ wu)) @ wd


def moe_swiglu(h, router_w, wg, wu, wd):
    bsz, s, d = h.shape
    t = h.reshape(bsz * s, d)
    logits = (t @ router_w).astype(jnp.float32)
    top_logits, top_idx = lax.top_k(logits, TOP_K)
    top_w = jax.nn.softmax(top_logits, axis=-1)
    gates = jnp.einsum('tk,tke->te', top_w, jax.nn.one_hot(top_idx, N_EXPERTS, dtype=jnp.float32)).astype(h.dtype)
    y = jnp.zeros_like(t)
    for e in range(N_EXPERTS):
        y = y + gates[:, e:e + 1] * swiglu(t, wg[e], wu[e], wd[e])
    return y.reshape(bsz, s, d)


def setup_inputs(seed: int = 0) -> dict:
    key = jax.random.key(seed)
    ks = iter(jax.random.split(key, 32))

    def nrm(shape, scale):
        return jax.random.normal(next(ks), shape, jnp.float32) * scale

    d = D_MODEL
    return {
        'x': nrm((BATCH, SEQ, d), 1.0),
        'c': nrm((BATCH, d), 1.0),
        'ada_w': nrm((DEPTH, d, 6 * d), 0.5 * d ** -0.5),
        'ada_b': nrm((DEPTH, 6 * d), 0.02),
        'norm1_w': 1.0 + nrm((DEPTH, d), 0.02),
        'norm2_w': 1.0 + nrm((DEPTH, d), 0.02),
        'final_norm_w': 1.0 + nrm((d,), 0.02),
        'm_w_in': nrm((N_MLSTM, d, M_PROJ), d ** -0.5),
        'm_i_bias': nrm((N_MLSTM, M_HEADS), 0.1),
        'm_f_bias': jnp.linspace(3.0, 6.0, M_HEADS, dtype=jnp.float32)[None, :] + nrm((N_MLSTM, M_HEADS), 0.1),
        'm_conv_w': nrm((N_MLSTM, CONV_K, 2 * M_QK), CONV_K ** -0.5),
        'm_conv_b': nrm((N_MLSTM, 2 * M_QK), 0.01),
        'm_norm_w': 1.0 + nrm((N_MLSTM, M_V), 0.02),
        'm_w_out': nrm((N_MLSTM, M_V, d), M_V ** -0.5),
        'h_w_in': nrm((N_HGRN, d, HG_PROJ), d ** -0.5),
        'h_lb_logits': nrm((N_HGRN, HG_HEADS * HG_DK), 0.5),
        'h_norm_w': 1.0 + nrm((N_HGRN, d), 0.02),
        'h_w_out': nrm((N_HGRN, d, d), d ** -0.5),
        'ffn_w_gate': nrm((N_MLSTM, d, D_FF), d ** -0.5),
        'ffn_w_up': nrm((N_MLSTM, d, D_FF), d ** -0.5),
        'ffn_w_down': nrm((N_MLSTM, D_FF, d), D_FF ** -0.5),
        'moe_router': nrm((N_HGRN, d, N_EXPERTS), d ** -0.5),
        'moe_w_gate': nrm((N_HGRN, N_EXPERTS, d, D_FF_EXPERT), d ** -0.5),
        'moe_w_up': nrm((N_HGRN, N_EXPERTS, d, D_FF_EXPERT), d ** -0.5),
        'moe_w_down': nrm((N_HGRN, N_EXPERTS, D_FF_EXPERT, d), D_FF_EXPERT ** -0.5),
    }


def reference(x, c, ada_w, ada_b, norm1_w, norm2_w, final_norm_w,
              m_w_in, m_i_bias, m_f_bias, m_conv_w, m_conv_b, m_norm_w, m_w_out,
              h_w_in, h_lb_logits, h_norm_w, h_w_out,
              ffn_w_gate, ffn_w_up, ffn_w_down,
              moe_router, moe_w_gate, moe_w_up, moe_w_down):
    cond = jax.nn.silu(c)
    gam = jax.nn.softmax(h_lb_logits.astype(jnp.float32), axis=0)
    lower_bounds = jnp.cumsum(gam, axis=0) - gam[:1]
    for layer in range(DEPTH):
        j = layer // N_MIXERS
        mod = (cond @ ada_w[layer] + ada_b[layer])[:, None, :]
        shift1, scale1, gate1, shift2, scale2, gate2 = jnp.split(mod, 6, axis=-1)
        hn = rmsnorm(x, norm1_w[layer]) * (1 + scale1) + shift1
        if layer % N_MIXERS == 0:
            y = mlstm_mixer(hn, m_w_in[j], m_i_bias[j], m_f_bias[j], m_conv_w[j], m_conv_b[j],
                            m_norm_w[j], m_w_out[j])
        else:
            y = hgrn2_mixer(hn, h_w_in[j], lower_bounds[j], h_norm_w[j], h_w_out[j])
        x = x + gate1 * y
        hn = rmsnorm(x, norm2_w[layer]) * (1 + scale2) + shift2
        if layer % 2 == 0:
            y = swiglu(hn, ffn_w_gate[j], ffn_w_up[j], ffn_w_down[j])
        else:
            y = moe_swiglu(hn, moe_router[j], moe_w_gate[j], moe_w_up[j], moe_w_down[j])
        x = x + gate2 * y
    return rmsnorm(x, final_norm_w)
```

```python
import os
import numpy as np
from contextlib import ExitStack
import concourse.bass as bass
import concourse.mybir as mybir
from concourse.bass_utils import run_bass_kernel_spmd

F32 = mybir.dt.float32
BF16 = mybir.dt.bfloat16
AF = mybir.ActivationFunctionType
ALU = mybir.AluOpType
AX = mybir.AxisListType

D = 1024
KC = 8
SEQ = 4096
TS = 512
NS = TS // 128
DEPTH = 4
M_PROJ = 3088
HG_PROJ = 4096
D_FF = 2816
NE = 8
D_FFE = 1408
EPS = 1e-6
DBG = float(os.environ.get("KDBG", "99"))
NSLOT = 6
NCAST = 3


class Buf:
    __slots__ = ("ap", "keys")

    def __init__(self, ap, keys):
        self.ap = ap
        self.keys = tuple(keys)


class Prog:
    ENG = ("pe", "act", "dve", "pool", "sp")

    def __init__(self):
        self.ops = {e: [] for e in self.ENG}
        self.tick = {e: 0 for e in self.ENG}
        self.chan = {}
        self.lastw = {}
        self.readers = {}
        self.seen = {e: {} for e in self.ENG}

    def _deps(self, eng, reads, writes):
        deps = {}

        def add(ev):
            if ev is None:
                return
            s, v = ev
            if deps.get(s, 0) < v:
                deps[s] = v

        for k in reads:
            add(self.lastw.get(k))
            if k[0] == "ps":
                for s, v in self.readers.get(k, {}).items():
                    if s != eng:
                        add((s, v))
        for k in writes:
            add(self.lastw.get(k))
            for s, v in self.readers.get(k, {}).items():
                add((s, v))
        waits = []
        seen = self.seen[eng]
        for s, v in deps.items():
            if s == "pe" and eng == "pe":
                continue
            if seen.get(s, 0) >= v:
                continue
            seen[s] = v
            waits.append((s, v))
        return waits

    def _commit(self, ev, reads, writes):
        s, v = ev
        for k in reads:
            self.readers.setdefault(k, {})[s] = v
        for k in writes:
            self.lastw[k] = ev
            self.readers[k] = {}

    def op(self, eng, fn, reads=(), writes=()):
        waits = self._deps(eng, reads, writes)
        self.tick[eng] += 1
        ev = (eng, self.tick[eng])
        self.ops[eng].append((fn, waits, ev, False))
        self._commit(ev, reads, writes)

    def dma(self, eng, fn, chan, reads=(), writes=()):
        writes = tuple(writes) + (("chan", chan),)
        waits = self._deps(eng, reads, writes)
        self.chan[chan] = self.chan.get(chan, 0) + 16
        ev = ("d:" + chan, self.chan[chan])
        self.ops[eng].append((fn, waits, ev, True))
        self._commit(ev, reads, writes)

    def emit(self, nc, final_chans):
        with ExitStack() as st:
            sems = {}
            for n in list(self.ENG) + ["d:" + c for c in self.chan]:
                sems[n] = st.enter_context(nc.semaphore("s_" + n.replace(":", "_")))
            block = st.enter_context(nc.Block())

            def run(name, e):
                for fn, waits, ev, isdma in self.ops[name]:
                    for s, v in waits:
                        e.wait_ge(sems[s], v)
                    ins = fn(e)
                    ins.then_inc(sems[ev[0]], 16 if isdma else 1)
                if name == "sp":
                    for c in final_chans:
                        e.wait_ge(sems["d:" + c], self.chan[c])

            @block.tensor
            def _(e):
                run("pe", e)

            @block.scalar
            def _(e):
                run("act", e)

            @block.vector
            def _(e):
                run("dve", e)

            @block.gpsimd
            def _(e):
                run("pool", e)

            @block.sync
            def _(e):
                run("sp", e)


def build(NT=8, NL=DEPTH, final=True, half="full"):
    nc = bass.Bass("TRN2", target_bir_lowering=False)
    P = Prog()
    st = ExitStack()

    def din(name, shape, dt=F32):
        return nc.dram_tensor(name, list(shape), dt, kind="ExternalInput").ap()

    x_d = din("x", [NT * TS, D])
    out_d = nc.dram_tensor("out", [NT * TS, D], F32, kind="ExternalOutput").ap()
    c_d = din("c", [128, KC])
    ada_w_d = din("ada_w", [DEPTH, D, 6 * D])
    ada_b_d = din("ada_b", [128, DEPTH, 48])
    n1_d = din("norm1_w", [128, DEPTH, KC])
    n2_d = din("norm2_w", [128, DEPTH, KC])
    fn_d = din("final_norm_w", [128, KC])
    gb_d = din("m_gate_bias", [128, 2, 16])
    cw_d = din("m_conv_w", [128, 2, KC, 4])
    cb_d = din("m_conv_b", [128, 2, KC])
    mnw_d = din("m_norm_w", [128, 2, KC])
    hnw_d = din("h_norm_w", [128, 2, KC])
    lb_d = din("h_lb_logits", [128, 2, KC])
    rt_d = din("moe_router", [128, 2, KC, NE])
    cst_d = din("consts", [128, 6, 128])
    wshapes = {
        "m_w_in": [2, D, M_PROJ], "m_w_out": [2, D, D], "h_w_in": [2, D, HG_PROJ], "h_w_out": [2, D, D],
        "ffn_w_gate": [2, D, D_FF], "ffn_w_up": [2, D, D_FF], "ffn_w_down": [2, D_FF, D],
        "moe_w_gate": [2, NE, D, D_FFE], "moe_w_up": [2, NE, D, D_FFE], "moe_w_down": [2, NE, D_FFE, D],
    }
    wsrc = {}
    wdst = {}
    for n, shp in wshapes.items():
        wsrc[n] = din(n, shp)
        wdst[n] = nc.dram_tensor(n + "_b", list(shp), BF16, kind="Internal").ap()

    def sb(name, shape, dt=F32):
        return st.enter_context(nc.sbuf_tensor(name, list(shape), dt))

    ps = st.enter_context(nc.psum_tensor("ps", [128, 8, 512], F32))

    def bank(i):
        return Buf(ps[:, i, :], [("ps", i)])

    xT = sb("xT", [128, KC, TS])
    tmp = sb("tmp", [128, KC, TS])
    hnT = sb("hnT", [128, KC, TS], BF16)
    hhnT = sb("hhnT", [128, KC, TS], BF16)
    sq = hhnT
    rstd = sb("rstd", [128, TS])
    ring = sb("ring", [128, NSLOT, 4096], BF16)
    qkT = sb("qkT", [128, KC, TS], BF16)
    khT = sb("khT", [128, KC, TS], BF16)
    gT = sb("gT", [128, KC, TS], BF16)
    vt = sb("vt", [128, NS, 8, 129], BF16)
    cv = sb("cv", [128, 2, TS + 3])
    acc = sb("acc", [128, 2, TS])
    ktm = sb("ktm", [128, 2, D], BF16)
    Sm = sb("Sm", [128, 2, 8, 128], BF16)
    hh = sb("hh", [128, 2, D], BF16)
    ft4 = sb("ft4", [128, 7, TS])
    sqn = ft4[:, 0:2, :].rearrange("p a n -> p (a n)")
    small = sb("small", [128, 20, 64])
    Cst = sb("Cst", [128, 2, 4, 129])
    Cbf = sb("Cbf", [128, 2, 2, 4, 129], BF16)
    Sst = sb("Sst", [128, 2, 8, 128])
    Sbf = sb("Sbf", [128, 4, 8, 128], BF16)
    amT = sb("amT", [128, 2, 8, 128], BF16)
    hist = sb("hist", [128, 2, KC, 3])
    cst = sb("cst", [128, 6, 128])
    cstb = sb("cstb", [128, 2, 128], BF16)
    cond = sb("cond", [128, KC])
    epsc = sb("epsc", [128, 1])
    l8c = sb("l8c", [128, 1])
    onec = sb("onec", [128, 1])
    modv = sb("modv", [128, DEPTH, 48])
    adabias = sb("adabias", [128, DEPTH, 48])
    n1 = sb("n1", [128, DEPTH, KC])
    n2 = sb("n2", [128, DEPTH, KC])
    fnw = sb("fnw", [128, KC])
    A1 = sb("A1", [128, DEPTH, KC])
    A2 = sb("A2", [128, DEPTH, KC])
    gbias = sb("gbias", [128, 2, 16])
    cw = sb("cw", [128, 2, KC, 4])
    cb = sb("cb", [128, 2, KC])
    mnw = sb("mnw", [128, 2, KC])
    hnw = sb("hnw", [128, 2, KC])
    lbl = sb("lbl", [128, 2, KC])
    lbv = sb("lbv", [128, 2, KC])
    omlb = sb("omlb", [128, 2, KC])
    rt = sb("rt", [128, 2, KC, NE])
    gbc = tmp

    def mm(out, lhsT, rhs, start=True, stop=True):
        o, l, r = out.ap, lhsT.ap, rhs.ap
        P.op("pe", lambda e: e.matmul(o, l, r, start=start, stop=stop),
             reads=lhsT.keys + rhs.keys, writes=out.keys)

    def tr(out, in_, ident):
        o, i, d = out.ap, in_.ap, ident.ap
        P.op("pe", lambda e: e.transpose(o, i, d), reads=in_.keys + ident.keys, writes=out.keys)

    def act(out, in_, func, scale=1.0, bias=0.0, extra=()):
        o, i = out.ap, in_.ap
        P.op("act", lambda e: e.activation(out=o, in_=i, func=func, scale=scale, bias=bias),
             reads=in_.keys + tuple(extra), writes=out.keys)

    def tt(out, a, b, op, eng="dve"):
        o, x0, x1 = out.ap, a.ap, b.ap
        P.op(eng, lambda e: e.tensor_tensor(out=o, in0=x0, in1=x1, op=op),
             reads=a.keys + b.keys, writes=out.keys)

    def tsc(out, a, s1, s2, op0, op1=None, extra=(), eng="dve"):
        o, x0 = out.ap, a.ap
        if op1 is None:
            P.op(eng, lambda e: e.tensor_scalar(out=o, in0=x0, scalar1=s1, scalar2=None, op0=op0),
                 reads=a.keys + tuple(extra), writes=out.keys)
        else:
            P.op(eng, lambda e: e.tensor_scalar(out=o, in0=x0, scalar1=s1, scalar2=s2, op0=op0, op1=op1),
                 reads=a.keys + tuple(extra), writes=out.keys)

    def stt(out, a, s, b, op0, op1, extra=(), eng="dve"):
        o, x0, x1 = out.ap, a.ap, b.ap
        P.op(eng, lambda e: e.scalar_tensor_tensor(out=o, in0=x0, scalar=s, in1=x1, op0=op0, op1=op1),
             reads=a.keys + b.keys + tuple(extra), writes=out.keys)

    def rsqrt_eps(out, in_, scale=1.0):
        act(out, in_, AF.Sqrt, scale=scale, bias=epsc[:out.ap.shape[0], 0:1], extra=[("epsc",)])
        o = out.ap
        P.op("dve", lambda e: e.reciprocal(out=o, in_=o), reads=out.keys, writes=out.keys)

    def cp(out, in_, eng="dve"):
        o, i = out.ap, in_.ap
        if eng == "act":
            P.op("act", lambda e: e.activation(out=o, in_=i, func=AF.Copy), reads=in_.keys, writes=out.keys)
        else:
            P.op(eng, lambda e: e.tensor_copy(out=o, in_=i), reads=in_.keys, writes=out.keys)

    def red(out, in_, op, eng="dve"):
        o, i = out.ap, in_.ap
        P.op(eng, lambda e: e.tensor_reduce(out=o, in_=i, axis=AX.X, op=op), reads=in_.keys, writes=out.keys)

    def memset(out, val, eng="dve"):
        o = out.ap
        P.op(eng, lambda e: e.memset(o, val), writes=out.keys)

    def load(dst, src_ap, chan, eng="sp", reads=()):
        o = dst.ap
        P.dma(eng, lambda e: e.dma_start(out=o, in_=src_ap), chan, reads=reads, writes=dst.keys)

    ring_ctr = [0]

    def ring_load(src_ap, shape, wkey):
        i = ring_ctr[0] % NSLOT
        ring_ctr[0] += 1
        n = int(np.prod(shape[1:]))
        if len(shape) == 3:
            view = ring[:, i, 0:n].rearrange("p (k n) -> p k n", k=shape[1])
        else:
            view = ring[:, i, 0:n]
        b = Buf(view, [("ring", i)])
        load(b, src_ap, "ring%d" % i, reads=[wkey])
        return b

    def wpiece(name, idx, c0, w):
        a = wdst[name]
        for i in idx:
            a = a[i]
        return a[:, c0:c0 + w].rearrange("(k p) n -> p k n", p=128)

    cast_ctr = [0]

    def cast(name, idx):
        s, d_ = wsrc[name], wdst[name]
        for i in idx:
            s, d_ = s[i], d_[i]
        sf = s.rearrange("r c -> (r c)").rearrange("(a b) -> a b", b=2048)
        df = d_.rearrange("r c -> (r c)").rearrange("(a b) -> a b", b=2048)
        ch = "cast%d" % (cast_ctr[0] % NCAST)
        cast_ctr[0] += 1
        P.dma("pool", lambda e: e.dma_start(out=df, in_=sf), ch, writes=[("wb", name) + tuple(idx)])

    for j in range(2):
        if 2 * j < NL:
            for n in ("m_w_in", "m_w_out", "ffn_w_gate", "ffn_w_up", "ffn_w_down"):
                cast(n, (j,))
        if 2 * j + 1 < NL:
            for n in ("h_w_in", "h_w_out"):
                cast(n, (j,))
            for e_ in range(NE):
                for n in ("moe_w_gate", "moe_w_up", "moe_w_down"):
                    cast(n, (j, e_))

    def K(name, *idx):
        return [(name,) + tuple(idx)] if idx else [(name,)]

    def cload(tile, src, name):
        load(Buf(tile[:], K(name)), src, "const")

    cload(cst, cst_d, "cst")
    cload(cond, c_d, "cond")
    cload(adabias, ada_b_d, "adabias")
    cload(n1, n1_d, "n1")
    cload(n2, n2_d, "n2")
    cload(fnw, fn_d, "fnw")
    cload(gbias, gb_d, "gbias")
    cload(cw, cw_d, "cw")
    cload(cb, cb_d, "cb")
    cload(mnw, mnw_d, "mnw")
    cload(hnw, hnw_d, "hnw")
    cload(lbl, lb_d, "lbl")
    cload(rt, rt_d, "rt")
    ident32 = Buf(cst[:, 0, :], K("cst"))
    mask32 = Buf(cst[:, 1, :], K("cst"))
    tripos = Buf(cst[:, 1, :], K("cst"))
    onespos = Buf(cst[:, 3, :], K("cst"))
    mask64 = Buf(cst[:, 5, :], K("cst"))
    cp(Buf(cstb[:, 0, :], K("cstb")), Buf(cst[:, 0, :], K("cst")))
    cp(Buf(cstb[:, 1, :], K("cstb")), Buf(cst[:, 4, :], K("cst")))
    identb = Buf(cstb[:, 0, :], K("cstb"))
    onesdiv = Buf(cstb[:, 1, :], K("cstb"))

    memset(Buf(epsc[:], [("epsc",)]), EPS)
    memset(Buf(l8c[:], [("l8c",)]), float(np.log(0.125)))
    memset(Buf(onec[:], [("onec",)]), 1.0)
    act(Buf(cond[:], K("cond")), Buf(cond[:], K("cond")), AF.Silu)
    memset(Buf(lbv[:, 0, :], K("lbv")), 0.0)
    tt(Buf(lbv[:, 1, :], K("lbv")), Buf(lbl[:, 1, :], K("lbl")), Buf(lbl[:, 0, :], K("lbl")), ALU.subtract)
    act(Buf(lbv[:, 1, :], K("lbv")), Buf(lbv[:, 1, :], K("lbv")), AF.Sigmoid)
    tsc(Buf(omlb[:], K("omlb")), Buf(lbv[:], K("lbv")), -1.0, 1.0, ALU.mult, ALU.add)
    memset(Buf(Cst[:], [("Cst", 0), ("Cst", 1)]), 0.0)
    memset(Buf(Cbf[:], [("Cbf", a_, b_) for a_ in range(2) for b_ in range(2)]), 0.0)
    memset(Buf(Sst[:], [("Sst", 0), ("Sst", 1)]), 0.0)
    memset(Buf(hist[:], K("hist")), 0.0)
    memset(Buf(small[:], [("sm", i_) for i_ in range(20)]), 0.0)
    memset(Buf(amT[:], [("amT", 0), ("amT", 1)]), 0.0)
    memset(Buf(vt[:], [("vt", s_) for s_ in range(NS)]), 1.0)
    memset(Buf(ft4[:, 6, :], K("ft4", 6)), 1.0)
    memset(Buf(ft4[:, 6, 0:TS:32], K("ft4", 6)), 0.0)

    for l in range(NL):
        for g in range(12):
            bslot = g % 2
            adab_v = ring[:, 2 * bslot:2 * bslot + 2, :].rearrange("p a n -> p (a n)").bitcast(F32).rearrange(
                "p (k n) -> p k n", k=KC)
            adk = [("ring", 2 * bslot), ("ring", 2 * bslot + 1)]
            dst = Buf(adab_v, adk)
            load(dst, ada_w_d[l, :, g * 512:(g + 1) * 512].rearrange("(k p) n -> p k n", p=128), "ada%d" % bslot)
            for nci in range(4):
                col = g * 4 + nci
                o = Buf(ps[:, 7, col:col + 1], [("ps", 7)])
                for k in range(KC):
                    mm(o, Buf(adab_v[:, k, nci * 128:(nci + 1) * 128], adk),
                       Buf(cond[:, k:k + 1], K("cond")), start=(k == 0), stop=(k == KC - 1))
        tt(Buf(modv[:, l, :], K("modv", l)), Buf(ps[:, 7, 0:48], [("ps", 7)]),
           Buf(adabias[:, l, :], K("adabias")), ALU.add)
        stt(Buf(A1[:, l, :], K("A1", l)), Buf(modv[:, l, 8:16], K("modv", l)), 1.0, Buf(n1[:, l, :], K("n1")),
            ALU.add, ALU.mult)
        stt(Buf(A2[:, l, :], K("A2", l)), Buf(modv[:, l, 32:40], K("modv", l)), 1.0, Buf(n2[:, l, :], K("n2")),
            ALU.add, ALU.mult)

    def shift1(l, c):
        return modv[:, l, 0 + c:1 + c]

    def gate1(l, c):
        return modv[:, l, 16 + c:17 + c]

    def shift2(l, c):
        return modv[:, l, 24 + c:25 + c]

    def gate2(l, c):
        return modv[:, l, 40 + c:41 + c]

    xk = [("xT", c) for c in range(KC)]
    hnk = [("hnT", c) for c in range(KC)]
    tmpk = [("tmp", c) for c in range(KC)]

    def rms_stats():
        for k in range(KC):
            if k % 2 == 0:
                act(Buf(sq[:, k, :], [("hhnT", k)]), Buf(xT[:, k, :], [("xT", k)]), AF.Square)
            else:
                tt(Buf(sq[:, k, :], [("hhnT", k)]), Buf(xT[:, k, :], [("xT", k)]), Buf(xT[:, k, :], [("xT", k)]), ALU.mult)
            mm(bank(0), onesdiv, Buf(sq[:, k, :], [("hhnT", k)]), start=(k == 0), stop=(k == KC - 1))
        rsqrt_eps(Buf(rstd[:], K("rstd")), bank(0))

    def norm_mod(l, which, keep32):
        rms_stats()
        A = A1 if which == 1 else A2
        for c in range(KC):
            tt(Buf(tmp[:, c, :], [("tmp", c)]), Buf(xT[:, c, :], [("xT", c)]), Buf(rstd[:], K("rstd")), ALU.mult)
            sh = shift1(l, c) if which == 1 else shift2(l, c)
            extra = K("A1" if which == 1 else "A2", l) + K("modv", l)
            if keep32:
                act(Buf(tmp[:, c, :], [("tmp", c)]), Buf(tmp[:, c, :], [("tmp", c)]), AF.Identity,
                    scale=A[:, l, c:c + 1], bias=sh, extra=extra)
                cp(Buf(hnT[:, c, :], [("hnT", c)]), Buf(tmp[:, c, :], [("tmp", c)]), eng="pool")
            else:
                act(Buf(hnT[:, c, :], [("hnT", c)]), Buf(tmp[:, c, :], [("tmp", c)]), AF.Identity,
                    scale=A[:, l, c:c + 1], bias=sh, extra=extra)

    def resid_add(dc, psb, gate_ap, l):
        stt(Buf(xT[:, dc, :], [("xT", dc)]), psb, gate_ap, Buf(xT[:, dc, :], [("xT", dc)]), ALU.mult, ALU.add,
            extra=K("modv", l))

    pbc = [0]

    def next_bank(lo, n):
        i = lo + pbc[0] % n
        pbc[0] += 1
        return i

    def proj_fm(slot, ncols, consume):
        for nci in range(ncols // 128):
            b = bank(next_bank(1, 2))
            for k in range(KC):
                mm(b, Buf(slot.ap[:, k, nci * 128:(nci + 1) * 128], slot.keys), Buf(hnT[:, k, :], [("hnT", k)]),
                   start=(k == 0), stop=(k == KC - 1))
            consume(nci, b)

    def out_proj(name, j, l):
        for g in range(2):
            slot = ring_load(wpiece(name, (j,), g * 512, 512), [128, KC, 512], ("wb", name, j))
            for nci in range(4):
                dc = g * 4 + nci
                b = bank(next_bank(1, 2))
                for k in range(KC):
                    mm(b, Buf(slot.ap[:, k, nci * 128:(nci + 1) * 128], slot.keys),
                       Buf(hhnT[:, k, :], [("hhnT", k)]), start=(k == 0), stop=(k == KC - 1))
                resid_add(dc, b, gate1(l, dc), l)

    def run_skewed(body):
        gens = [body(s_) for s_ in range(NS)]
        next(gens[0])
        for s_ in range(NS):
            next(gens[s_])
            if s_ + 1 < NS:
                next(gens[s_ + 1])
            for _ in gens[s_]:
                pass

    def mlstm(l, ti):
        j = l // 2
        wk = ("wb", "m_w_in", j)
        slot = ring_load(wpiece("m_w_in", (j,), 3072, 16), [128, KC, 16], wk)
        gps = Buf(ps[:, 7, 0:NS * 16].rearrange("p (s n) -> p s n", s=NS), [("ps", 7)])
        for s in range(NS):
            o = Buf(ps[:, 7, s * 16:(s + 1) * 16], [("ps", 7)])
            for k in range(KC):
                mm(o, Buf(hnT[:, k, s * 128:(s + 1) * 128], [("hnT", k)]), Buf(slot.ap[:, k, :], slot.keys),
                   start=(k == 0), stop=(k == KC - 1))
        G = small[:, 0, 0:NS * 16].rearrange("p (s n) -> p s n", s=NS)
        tt(Buf(G, K("sm", 0)), gps, Buf(gbias[:, j, :].unsqueeze(1).to_broadcast([128, NS, 16]), K("gbias")),
           ALU.add)
        if DBG <= 2.2:
            return
        th = small[:, 1, 0:NS * 8].rearrange("p (s n) -> p s n", s=NS)
        act(Buf(th, K("sm", 1)), Buf(G[:, :, 0:8], K("sm", 0)), AF.Tanh, scale=1.0 / 15.0)
        spv = small[:, 2, 0:NS * 8].rearrange("p (s n) -> p s n", s=NS)
        act(Buf(spv, K("sm", 2)), Buf(G[:, :, 8:16], K("sm", 0)), AF.Exp, scale=-1.0)
        act(Buf(spv, K("sm", 2)), Buf(spv, K("sm", 2)), AF.Ln, bias=1.0)
        if DBG <= 2.4:
            return
        bps = ps[:, 7, 64:64 + NS * 16].rearrange("p (s n) -> p s n", s=NS)
        for s in range(NS):
            mm(Buf(ps[:, 7, 64 + s * 16:64 + s * 16 + 8], [("ps", 7)]), tripos, Buf(spv[:, s, :], K("sm", 2)))
            mm(Buf(ps[:, 7, 64 + s * 16 + 8:64 + s * 16 + 16], [("ps", 7)]), onespos, Buf(spv[:, s, :], K("sm", 2)))
        if DBG <= 2.6:
            return
        bps_b = Buf(bps, [("ps", 7)])
        eb = small[:, 3, 0:NS * 16].rearrange("p (s n) -> p s n", s=NS)
        act(Buf(eb, K("sm", 3)), bps_b, AF.Exp, scale=-1.0)
        if DBG <= 2.7:
            return
        wv = small[:, 4, 0:NS * 8].rearrange("p (s n) -> p s n", s=NS)
        bsb = small[:, 11, 0:NS * 16].rearrange("p (s n) -> p s n", s=NS)
        cp(Buf(bsb, K("sm", 11)), bps_b)
        stt(Buf(wv, K("sm", 4)), Buf(th, K("sm", 1)), 15.0, Buf(bsb[:, :, 0:8], K("sm", 11)), ALU.mult, ALU.add)
        act(Buf(wv, K("sm", 4)), Buf(wv, K("sm", 4)), AF.Exp, bias=l8c[:, 0:1], extra=[("l8c",)])
        if DBG <= 2.8:
            return
        EL = small[:, 5, 0:NS * 4].rearrange("p (s n) -> p s n", s=NS)
        cp(Buf(EL[0:64], K("sm", 5)), Buf(eb[0:64, :, 8:16:2], K("sm", 3)))
        cp(Buf(EL[64:128], K("sm", 5)), Buf(eb[64:128, :, 9:16:2], K("sm", 3)))

        if DBG <= 3:
            return
        pend_silu = []
        for qi in range(2):
            slot = ring_load(wpiece("m_w_in", (j,), qi * 512, 512), [128, KC, 512], wk)

            def consume(nci, b, qi=qi):
                c = qi * 4 + nci
                cb_ = c % 2
                cvb = Buf(cv[:, cb_, :], [("cv", cb_)])
                act(Buf(cv[:, cb_, 3:3 + TS], [("cv", cb_)]), b, AF.Copy)
                while pend_silu:
                    pend_silu.pop(0)()
                cp(Buf(cv[:, cb_, 0:3], [("cv", cb_)]), Buf(hist[:, j, c, :], K("hist")), eng="pool")
                ab = Buf(acc[:, cb_, :], [("acc", cb_)])
                tsc(ab, Buf(cv[:, cb_, 0:TS], cvb.keys), cw[:, j, c, 0:1], cb[:, j, c:c + 1], ALU.mult, ALU.add,
                    extra=K("cw") + K("cb"))
                for tap in range(1, 4):
                    stt(ab, Buf(cv[:, cb_, tap:tap + TS], cvb.keys), cw[:, j, c, tap:tap + 1], ab, ALU.mult, ALU.add,
                        extra=K("cw"))
                cp(Buf(hist[:, j, c, :], K("hist")), Buf(cv[:, cb_, TS:TS + 3], cvb.keys), eng="pool")
                pend_silu.append(lambda c=c, ab=ab: act(Buf(qkT[:, c, :], [("qk", c)]), ab, AF.Silu))

            proj_fm(slot, 512, consume)
        while pend_silu:
            pend_silu.pop(0)()
        if DBG <= 4:
            return
        for oi in range(2):
            slot = ring_load(wpiece("m_w_in", (j,), 2048 + oi * 512, 512), [128, KC, 512], wk)

            def consume(nci, b, oi=oi):
                c = oi * 4 + nci
                act(Buf(gT[:, c, :], [("gT", c)]), b, AF.Sigmoid)
                tsc(Buf(gT[:, c, :], [("gT", c)]), Buf(gT[:, c, :], [("gT", c)]), mnw[:, j, c:c + 1], None, ALU.mult,
                    extra=K("mnw"))

            proj_fm(slot, 512, consume)
        for vi in range(2):
            slot = ring_load(wpiece("m_w_in", (j,), 1024 + vi * 512, 512), [128, KC, 512], wk)
            for s in range(NS):
                b = bank(next_bank(1, 2))
                for k in range(KC):
                    mm(b, Buf(hnT[:, k, s * 128:(s + 1) * 128], [("hnT", k)]), Buf(slot.ap[:, k, :], slot.keys),
                       start=(k == 0), stop=(k == KC - 1))
                tt(Buf(vt[:, s, vi * 4:(vi + 1) * 4, 0:128], [("vt", s)]),
                   Buf(b.ap.rearrange("p (h n) -> p h n", h=4), b.keys),
                   Buf(wv[:, s, vi * 4:(vi + 1) * 4].unsqueeze(2).to_broadcast([128, 4, 128]), K("sm", 4)), ALU.mult)
        for s in range(NS):
            cp(Buf(vt[:, s, :, 128], [("vt", s)]), Buf(wv[:, s, :], K("sm", 4)))

        if DBG <= 5:
            return
        def body(s):
            gi = ti * NS + s
            pb_ = gi % 2
            t0 = s * 128
            ktp = Buf(ps[:, 0, :].bitcast(BF16)[:, 0:512], [("ps", 0)])
            for c in range(4):
                tr(Buf(ps[:, 0, :].bitcast(BF16)[:, c * 128:(c + 1) * 128], [("ps", 0)]),
                   Buf(qkT[:, 4 + c, t0:t0 + 128], [("qk", 4 + c)]), identb)
            kb = Buf(ktm[:, pb_, 0:512], [("ktm", pb_)])
            cp(kb, ktp, eng="act")
            stb = Buf(ps[:, 1:3, :].rearrange("p b n -> p (b n)"), [("ps", 1), ("ps", 2)])
            for h in range(8):
                c, po = h // 2, (h % 2) * 64
                mm(Buf(ps[:, 1 + h % 2, (h // 2) * 128:(h // 2 + 1) * 128], stb.keys),
                   Buf(qkT[po:po + 64, 4 + c, t0:t0 + 128], [("qk", 4 + c)]),
                   Buf(qkT[po:po + 64, c, t0:t0 + 128], [("qk", c)]))
            smb = Buf(Sm[:, pb_], [("Sm", pb_)])
            tt(Buf(Sm[:, pb_].rearrange("p (pr par) n -> p par pr n", par=2), smb.keys),
               Buf(ps[:, 1:3, :].rearrange("p b (q n) -> p b q n", q=4), stb.keys),
               Buf(mask32.ap.unsqueeze(1).unsqueeze(1).to_broadcast([128, 2, 4, 128]), mask32.keys), ALU.mult)
            dck = [("ps", 6), ("ps", 7)]
            for h in range(8):
                pr, po = h // 2, (h % 2) * 64
                mm(Buf(ps[po:po + 64, 6 + pr // 2, (pr % 2) * 256:(pr % 2) * 256 + 129], dck),
                   Buf(ktm[:, pb_, h * 64:(h + 1) * 64], kb.keys), Buf(vt[:, s, h, :], [("vt", s)]))
            cprev = gi % 2
            dcv = Buf(ps[:, 6:8, :].rearrange("p b (q n) -> p (b q) n", q=2)[:, :, 0:129], dck)
            cs = Buf(Cst[:, j], K("Cst", j))
            tt(cs, cs, dcv, ALU.add)
            tt(cs, cs, Buf(EL[:, s, :].unsqueeze(2).to_broadcast([128, 4, 129]), K("sm", 5)), ALU.mult)
            cp(Buf(Cbf[:, j, 1 - cprev], [("Cbf", j, 1 - cprev)]), cs, eng="pool")
            yield
            numk = [("ps", 3), ("ps", 4)]
            denk = [("ps", 5)]
            cprev = gi % 2
            for h in range(8):
                c, po, pr = h // 2, (h % 2) * 64, h // 2
                o = Buf(ps[:, 3 + h // 4, (h % 4) * 128:(h % 4 + 1) * 128], numk)
                mm(o, Buf(Sm[:, pb_, h, :], smb.keys), Buf(vt[:, s, h, 0:128], [("vt", s)]), start=True, stop=False)
                mm(o, Buf(qkT[po:po + 64, c, t0:t0 + 128], [("qk", c)]),
                   Buf(Cbf[po:po + 64, j, cprev, pr, 0:128], [("Cbf", j, cprev)]), start=False, stop=True)
            for h in range(8):
                c, po, pr = h // 2, (h % 2) * 64, h // 2
                o = Buf(ps[:, 5, h:h + 1], denk)
                mm(o, Buf(Sm[:, pb_, h, :], smb.keys), Buf(vt[:, s, h, 128:129], [("vt", s)]), start=True, stop=False)
                mm(o, Buf(qkT[po:po + 64, c, t0:t0 + 128], [("qk", c)]),
                   Buf(Cbf[po:po + 64, j, cprev, pr, 128:129], [("Cbf", j, cprev)]), start=False, stop=True)
            numv = Buf(ps[:, 3:5, :].rearrange("p b (q n) -> p (b q) n", q=4), numk)
            den = Buf(ps[:, 5, 0:8], denk)
            ebs = Buf(eb[:, s, 0:8], K("sm", 3))
            r0 = Buf(small[:, 6, 0:8], K("sm", 6))
            r1 = Buf(small[:, 7, 0:8], K("sm", 7))
            r2 = Buf(small[:, 8, 0:8], K("sm", 8))
            r3 = Buf(small[:, 9, 0:8], K("sm", 9))
            act(r0, den, AF.Abs)
            tt(r0, r0, ebs, ALU.mult)
            tsc(r0, r0, 1.0, None, ALU.max)
            P.op("dve", lambda e, o=r1.ap, i=r0.ap: e.reciprocal(out=o, in_=i), reads=r0.keys, writes=r1.keys)
            tt(r1, r1, ebs, ALU.mult)
            sqb = Buf(sqn.rearrange("p (h n) -> p h n", h=8), [("ft4", 0), ("ft4", 1)])
            act(sqb, numv, AF.Square)
            red(r2, sqb, ALU.add)
            tt(r3, r1, r1, ALU.mult)
            tt(r2, r2, r3, ALU.mult)
            rsqrt_eps(r2, r2, scale=1.0 / 128.0)
            tt(r2, r2, r1, ALU.mult)
            hb = Buf(hh[:, pb_, :], [("hh", pb_)])
            tt(Buf(hh[:, pb_, :].rearrange("p (h n) -> p h n", h=8), hb.keys), numv,
               Buf(r2.ap.unsqueeze(2).to_broadcast([128, 8, 128]), r2.keys), ALU.mult)
            yield
            tpb = Buf(ps[:, 0, :].bitcast(BF16), [("ps", 0)])
            for c in range(8):
                tr(Buf(ps[:, 0, :].bitcast(BF16)[:, c * 128:(c + 1) * 128], [("ps", 0)]),
                   Buf(hh[:, pb_, c * 128:(c + 1) * 128], hb.keys), identb)
            tt(Buf(hhnT[:, :, t0:t0 + 128], [("hhnT", c) for c in range(8)]),
               Buf(tpb.ap.rearrange("p (c n) -> p c n", c=8), tpb.keys),
               Buf(gT[:, :, t0:t0 + 128], [("gT", c) for c in range(8)]), ALU.mult)
        run_skewed(body)
        out_proj("m_w_out", j, l)

    def hgrn(l, ti):
        j = l // 2
        wk = ("wb", "h_w_in", j)
        CH = 32
        NCH = TS // CH
        CPS = 128 // CH

        def sm2(i):
            return small[:, i:i + 2, :].rearrange("p a n -> p (a n)").rearrange("p (h c) -> p h c", h=8)

        Bref, BLv, eref, eL, eLR = sm2(10), sm2(12), sm2(14), sm2(16), sm2(18)
        kB, kBL, kER, kEL, kELR = (K("sm", 10) + K("sm", 11), K("sm", 12) + K("sm", 13), K("sm", 14) + K("sm", 15),
                                   K("sm", 16) + K("sm", 17), K("sm", 18) + K("sm", 19))
        slots_q = [ring_load(wpiece("h_w_in", (j,), qi * 512, 512), [128, KC, 512], wk) for qi in range(2)]
        slots_f = [None, None]
        for h in range(8):
            if h % 4 == 0:
                slots_f[h // 4] = ring_load(wpiece("h_w_in", (j,), 1024 + (h // 4) * 512, 512), [128, KC, 512], wk)
            sq_ = slots_q[h // 4]
            sf_ = slots_f[h // 4]
            nci = h % 4
            bq = bank(next_bank(1, 2))
            for k in range(KC):
                mm(bq, Buf(sq_.ap[:, k, nci * 128:(nci + 1) * 128], sq_.keys), Buf(hnT[:, k, :], [("hnT", k)]),
                   start=(k == 0), stop=(k == KC - 1))
            qb = Buf(acc[:, h % 2, :], [("acc", h % 2)])
            act(qb, bq, AF.Silu)
            bf_ = bank(next_bank(1, 2))
            for k in range(KC):
                mm(bf_, Buf(sf_.ap[:, k, nci * 128:(nci + 1) * 128], sf_.keys), Buf(hnT[:, k, :], [("hnT", k)]),
                   start=(k == 0), stop=(k == KC - 1))
            fb = 3 * (h % 2)
            f0 = Buf(ft4[:, fb + 0, :], K("ft4", fb + 0))
            f1 = Buf(ft4[:, fb + 1, :], K("ft4", fb + 1))
            f2 = Buf(ft4[:, fb + 2, :], K("ft4", fb + 2))
            act(f0, bf_, AF.Sigmoid)
            act(f0, f0, AF.Identity, scale=omlb[:, j, h:h + 1], bias=lbv[:, j, h:h + 1], extra=K("omlb") + K("lbv"))
            tsc(f0, f0, 1e-30, None, ALU.max)
            act(f1, f0, AF.Ln)
            P.op("dve", lambda e, o_=f2.ap, a_=f1.ap, m_=ft4[:, 6, :]: e.tensor_tensor_scan(
                out=o_, data0=m_, data1=a_, initial=0.0, op0=ALU.mult, op1=ALU.add),
                reads=f1.keys + tuple(K("ft4", 6)), writes=f2.keys)
            B3 = f2.ap.rearrange("p (c n) -> p c n", n=CH)
            cp(Buf(Bref[:, h, :], kB), Buf(B3[:, :, CH // 2 - 1], f2.keys), eng="pool")
            cp(Buf(BLv[:, h, :], kBL), Buf(B3[:, :, CH - 1], f2.keys), eng="pool")
            act(f0, f0, AF.Identity, scale=-1.0, bias=onec[:, 0:1], extra=[("onec",)])
            tt(Buf(B3, f2.keys), Buf(B3, f2.keys),
               Buf(Bref[:, h, :].unsqueeze(2).to_broadcast([128, NCH, CH]), kB), ALU.subtract)
            act(f1, f2, AF.Exp)
            tt(Buf(qkT[:, h, :], [("qk", h)]), qb, f1, ALU.mult)
            act(f1, f2, AF.Exp, scale=-1.0)
            tt(Buf(khT[:, h, :], [("kh", h)]), f0, f1, ALU.mult)
        act(Buf(eref, kER), Buf(Bref, kB), AF.Exp)
        act(Buf(eL, kEL), Buf(BLv, kBL), AF.Exp)
        tt(Buf(eLR, kELR), Buf(BLv, kBL), Buf(Bref, kB), ALU.subtract)
        act(Buf(eLR, kELR), Buf(eLR, kELR), AF.Exp)
        for gi_ in range(2):
            slot = ring_load(wpiece("h_w_in", (j,), 3072 + gi_ * 512, 512), [128, KC, 512], wk)

            def consume(nci, b, gi_=gi_):
                c = gi_ * 4 + nci
                act(Buf(gT[:, c, :], [("gT", c)]), b, AF.Silu)
                tsc(Buf(gT[:, c, :], [("gT", c)]), Buf(gT[:, c, :], [("gT", c)]), hnw[:, j, c:c + 1], None, ALU.mult,
                    extra=K("hnw"))

            proj_fm(slot, 512, consume)
        for vi in range(2):
            slot = ring_load(wpiece("h_w_in", (j,), 2048 + vi * 512, 512), [128, KC, 512], wk)
            for s in range(NS):
                b = bank(next_bank(1, 2))
                for k in range(KC):
                    mm(b, Buf(hnT[:, k, s * 128:(s + 1) * 128], [("hnT", k)]), Buf(slot.ap[:, k, :], slot.keys),
                       start=(k == 0), stop=(k == KC - 1))
                P.op("act", lambda e, o=vt[:, s, vi * 4:(vi + 1) * 4, 0:128],
                     i=b.ap.rearrange("p (h n) -> p h n", h=4): e.activation(out=o, in_=i, func=AF.Copy),
                     reads=b.keys, writes=[("vt", s)])

        def mmt(out, lhsT, rhs, start, stop, tp):
            o, l_, r_ = out.ap, lhsT.ap, rhs.ap
            P.op("pe", lambda e: e.matmul(o, l_, r_, start=start, stop=stop, tile_position=tp, skip_group_check=True),
                 reads=lhsT.keys + rhs.keys, writes=out.keys)

        def body(s):
            gi = ti * NS + s
            pb_ = gi % 2
            t0 = s * 128
            ke = Buf(Sm[:, pb_], [("Sm", pb_)])
            tt(Buf(Sm[:, pb_].rearrange("p h (c n) -> p h c n", c=CPS), ke.keys),
               Buf(khT[:, :, t0:t0 + 128].rearrange("p h (c n) -> p h c n", c=CPS), [("kh", h) for h in range(8)]),
               Buf(eLR[:, :, CPS * s:CPS * s + CPS].unsqueeze(3).to_broadcast([128, 8, CPS, CH]), kELR), ALU.mult)
            tpb = Buf(ps[:, 0, :].bitcast(BF16), [("ps", 0)])
            for h in range(8):
                tr(Buf(ps[:, 0, :].bitcast(BF16)[:, h * 128:(h + 1) * 128], [("ps", 0)]),
                   Buf(Sm[:, pb_, h, :], ke.keys), identb)
            kb = Buf(ktm[:, pb_, :], [("ktm", pb_)])
            cp(kb, tpb, eng="act")
            atk = [("ps", 7)]
            for h in range(8):
                for c in range(CPS):
                    r0 = c * CH
                    mmt(Buf(ps[r0:r0 + CH, 7, h * CH:(h + 1) * CH], atk),
                        Buf(khT[:, h, t0 + r0:t0 + r0 + CH], [("kh", h)]),
                        Buf(qkT[:, h, t0 + r0:t0 + r0 + CH], [("qk", h)]), True, True, (0, r0))
            amk = [("amT", pb_)]
            for c in range(CPS):
                r0 = c * CH
                tt(Buf(amT[r0:r0 + CH, pb_, :, r0:r0 + CH], amk),
                   Buf(ps[r0:r0 + CH, 7, 0:8 * CH].rearrange("p (h n) -> p h n", h=8), atk),
                   Buf(cst[r0:r0 + CH, 5, 0:CH].unsqueeze(1).to_broadcast([CH, 8, CH]), K("cst")), ALU.mult)
            sall = Buf(Sst[:, j], K("Sst", j))
            for c in range(CPS):
                gc = CPS * s + c
                r0 = c * CH
                sbk = [("Sbf", c)]
                db = 5 if c % 2 == 0 else 1
                dck = [("ps", db), ("ps", db + 1)]
                for h in range(8):
                    act(Buf(Sbf[:, c, h, :], sbk), Buf(Sst[:, j, h, :], K("Sst", j)), AF.Copy,
                        scale=eref[:, h, gc:gc + 1], extra=kER)
                for h in range(8):
                    mmt(Buf(ps[:, db + h // 4, (h % 4) * 128:(h % 4 + 1) * 128], dck),
                        Buf(ktm[r0:r0 + CH, pb_, h * 128:(h + 1) * 128], kb.keys),
                        Buf(vt[r0:r0 + CH, s, h, 0:128], [("vt", s)]), True, True, (r0, 0))
                tt(sall, sall, Buf(eL[:, :, gc:gc + 1].to_broadcast([128, 8, 128]), kEL), ALU.mult)
                tt(sall, sall, Buf(ps[:, db:db + 2, :].rearrange("p b (q n) -> p (b q) n", q=4), dck), ALU.add)
            yield
            numk = [("ps", 3), ("ps", 4)]
            for h in range(8):
                o = ps[:, 3 + h // 4, (h % 4) * 128:(h % 4 + 1) * 128]
                mmt(Buf(o, numk), Buf(amT[:, pb_, h, :], amk), Buf(vt[:, s, h, 0:128], [("vt", s)]), True, False, None)
                for c in range(CPS):
                    r0 = c * CH
                    mmt(Buf(ps[r0:r0 + CH, 3 + h // 4, (h % 4) * 128:(h % 4 + 1) * 128], numk),
                        Buf(qkT[:, h, t0 + r0:t0 + r0 + CH], [("qk", h)]), Buf(Sbf[:, c, h, :], [("Sbf", c)]),
                        False, True, (0, r0))
            ov = Buf(ps[:, 3:5, :].rearrange("p b (q n) -> p (b q) n", q=4), numk)
            sqb = Buf(sqn.rearrange("p (h n) -> p h n", h=8), [("ft4", 0), ("ft4", 1)])
            r2 = Buf(small[:, 8, 0:8], K("sm", 8))
            act(sqb, ov, AF.Square)
            red(r2, sqb, ALU.add)
            rsqrt_eps(r2, r2, scale=1.0 / 128.0)
            hb = Buf(hh[:, pb_, :], [("hh", pb_)])
            tt(Buf(hh[:, pb_, :].rearrange("p (h n) -> p h n", h=8), hb.keys), ov,
               Buf(r2.ap.unsqueeze(2).to_broadcast([128, 8, 128]), r2.keys), ALU.mult)
            yield
            for c in range(8):
                tr(Buf(ps[:, 0, :].bitcast(BF16)[:, c * 128:(c + 1) * 128], [("ps", 0)]),
                   Buf(hh[:, pb_, c * 128:(c + 1) * 128], hb.keys), identb)
            tt(Buf(hhnT[:, :, t0:t0 + 128], [("hhnT", c) for c in range(8)]),
               Buf(tpb.ap.rearrange("p (c n) -> p c n", c=8), tpb.keys),
               Buf(gT[:, :, t0:t0 + 128], [("gT", c) for c in range(8)]), ALU.mult)
        run_skewed(body)
        out_proj("h_w_out", j, l)

    def ffn_groups(gname, uname, dname, idx, dff, gate_idx=None):
        out = []
        c0 = 0
        while c0 < dff:
            w = min(512, dff - c0)
            out.append((gname, uname, dname, tuple(idx), c0, w, gate_idx))
            c0 += w
        return out

    ffn_ctr = [0]

    def ffn_run(l, groups):
        pend = None

        def down(sd_, hb_i, ncn):
            for dc in range(KC):
                by = bank(5 + dc % 2)
                for nci in range(ncn):
                    mm(by, Buf(sd_.ap[:, nci, dc * 128:(dc + 1) * 128], sd_.keys),
                       Buf(qkT[:, hb_i * 4 + nci, :], [("qk", hb_i * 4 + nci)]), start=(nci == 0), stop=(nci == ncn - 1))
                resid_add(dc, by, gate2(l, dc), l)

        for (gname, uname, dname, idx, c0, w, gate_idx) in groups:
            ncn = w // 128
            sg_ = ring_load(wpiece(gname, idx, c0, w), [128, KC, w], ("wb", gname) + idx)
            su_ = ring_load(wpiece(uname, idx, c0, w), [128, KC, w], ("wb", uname) + idx)
            hb_i = ffn_ctr[0] % 2
            ffn_ctr[0] += 1
            for nci in range(ncn):
                bg = bank(1 + (nci % 2) * 2)
                bu = bank(2 + (nci % 2) * 2)
                for k in range(KC):
                    mm(bg, Buf(sg_.ap[:, k, nci * 128:(nci + 1) * 128], sg_.keys), Buf(hnT[:, k, :], [("hnT", k)]),
                       start=(k == 0), stop=(k == KC - 1))
                for k in range(KC):
                    mm(bu, Buf(su_.ap[:, k, nci * 128:(nci + 1) * 128], su_.keys), Buf(hnT[:, k, :], [("hnT", k)]),
                       start=(k == 0), stop=(k == KC - 1))
                sgb = Buf(ft4[:, nci % 2, :], K("ft4", nci % 2))
                act(sgb, bg, AF.Silu)
                hk = [("qk", hb_i * 4 + nci)]
                if gate_idx is not None:
                    tt(sgb, sgb, Buf(gbc[:, gate_idx, :], [("tmp", gate_idx)]), ALU.mult, eng="pool")
                tt(Buf(qkT[:, hb_i * 4 + nci, :], hk), sgb, bu, ALU.mult)
            if pend is not None:
                down(*pend)
            a_ = wdst[dname]
            for i in idx:
                a_ = a_[i]
            sd_ = ring_load(a_[c0:c0 + w, :].rearrange("(k p) n -> p k n", p=128), [128, ncn, D], ("wb", dname) + idx)
            pend = (sd_, hb_i, ncn)
        if pend is not None:
            down(*pend)

    def moe(l, ti):
        j = l // 2
        lg = Buf(ps[:, 7, 0:NS * 8].rearrange("p (s n) -> p s n", s=NS), [("ps", 7)])
        for s in range(NS):
            o = Buf(ps[:, 7, s * 8:(s + 1) * 8], [("ps", 7)])
            for k in range(KC):
                mm(o, Buf(tmp[:, k, s * 128:(s + 1) * 128], [("tmp", k)]), Buf(rt[:, j, k, :], K("rt")),
                   start=(k == 0), stop=(k == KC - 1))
        L = Buf(small[:, 0, 0:NS * 8].rearrange("p (s n) -> p s n", s=NS), K("sm", 0))
        cp(L, lg)
        m1 = Buf(small[:, 1, 0:NS], K("sm", 1))
        m2 = Buf(small[:, 2, 0:NS], K("sm", 2))
        eq = Buf(small[:, 3, 0:NS * 8].rearrange("p (s n) -> p s n", s=NS), K("sm", 3))
        pe_ = Buf(small[:, 4, 0:NS * 8].rearrange("p (s n) -> p s n", s=NS), K("sm", 4))
        red(m1, L, ALU.max)
        tt(eq, L, Buf(m1.ap.unsqueeze(2).to_broadcast([128, NS, 8]), m1.keys), ALU.is_equal)
        stt(eq, eq, -1e30, L, ALU.mult, ALU.add)
        red(m2, eq, ALU.max)
        tt(eq, L, Buf(m2.ap.unsqueeze(2).to_broadcast([128, NS, 8]), m2.keys), ALU.is_ge)
        tt(pe_, L, Buf(m1.ap.unsqueeze(2).to_broadcast([128, NS, 8]), m1.keys), ALU.subtract)
        act(pe_, pe_, AF.Exp)
        tt(pe_, pe_, eq, ALU.mult)
        red(m2, pe_, ALU.add)
        P.op("dve", lambda e, o=m2.ap, i=m2.ap: e.reciprocal(out=o, in_=i), reads=m2.keys, writes=m2.keys)
        tt(pe_, pe_, Buf(m2.ap.unsqueeze(2).to_broadcast([128, NS, 8]), m2.keys), ALU.mult)
        for e_ in range(NE):
            b = bank(next_bank(1, 2))
            for s in range(NS):
                mm(Buf(b.ap[:, s * 128:(s + 1) * 128], b.keys),
                   Buf(pe_.ap[:, s, e_:e_ + 1].to_broadcast([128, 128]), pe_.keys), ident32)
            P.op("act", lambda e, o=gbc[:, e_, :], i=b.ap: e.activation(out=o, in_=i, func=AF.Copy),
                 reads=b.keys, writes=[("tmp", e_)])
        groups = []
        for e_ in range(NE):
            groups += ffn_groups("moe_w_gate", "moe_w_up", "moe_w_down", (j, e_), D_FFE, gate_idx=e_)
        ffn_run(l, groups)

    for ti in range(NT):
        r0 = ti * TS
        xin = tmp[:].rearrange("p c t -> p (c t)").rearrange("p (s d) -> p s d", s=NS)
        load(Buf(xin, tmpk), x_d[r0:r0 + TS, :].rearrange("(s p) d -> p s d", p=128), "xin")
        for c in range(KC):
            b = bank(next_bank(1, 2))
            for s in range(NS):
                tr(Buf(b.ap[:, s * 128:(s + 1) * 128], b.keys), Buf(xin[:, s, c * 128:(c + 1) * 128], tmpk), ident32)
            P.op("act", lambda e, o=xT[:, c, :], i=b.ap: e.activation(out=o, in_=i, func=AF.Copy),
                 reads=b.keys, writes=[("xT", c)])
        for l in range(NL):
            if DBG <= 1:
                break
            norm_mod(l, 1, False)
            if DBG <= 2:
                break
            if l % 2 == 0:
                mlstm(l, ti)
            else:
                hgrn(l, ti)
            if half == "mixer" and l == NL - 1:
                break
            if l % 2 == 0:
                norm_mod(l, 2, False)
                ffn_run(l, ffn_groups("ffn_w_gate", "ffn_w_up", "ffn_w_down", (l // 2,), D_FF))
            else:
                norm_mod(l, 2, True)
                moe(l, ti)
        if final:
            rms_stats()
            tt(Buf(tmp[:], tmpk), Buf(xT[:], xk), Buf(rstd[:].unsqueeze(1).to_broadcast([128, KC, TS]), K("rstd")),
               ALU.mult)
            for c in range(KC):
                tsc(Buf(tmp[:, c, :], [("tmp", c)]), Buf(tmp[:, c, :], [("tmp", c)]), fnw[:, c:c + 1], None, ALU.mult,
                    extra=K("fnw"))
            src, srck, stg, stgk = tmp, "tmp", xT, xk
        else:
            src, srck, stg, stgk = xT, "xT", tmp, tmpk
        yout = stg[:].rearrange("p c t -> p (c t)").rearrange("p (s d) -> p s d", s=NS)
        for s in range(NS):
            for half in range(2):
                b = bank(next_bank(1, 2))
                for cc in range(4):
                    c = half * 4 + cc
                    tr(Buf(b.ap[:, cc * 128:(cc + 1) * 128], b.keys), Buf(src[:, c, s * 128:(s + 1) * 128], [(srck, c)]),
                       ident32)
                P.op("act", lambda e, o=yout[:, s, half * 512:(half + 1) * 512], i=b.ap: e.activation(
                    out=o, in_=i, func=AF.Copy), reads=b.keys, writes=stgk)
        P.dma("sp", lambda e, o=out_d[r0:r0 + TS, :].rearrange("(s p) d -> p s d", p=128), i=yout: e.dma_start(
            out=o, in_=i), "out", reads=stgk)

    fch = ["out"]
    if os.environ.get("KDUMP"):
        dbg_d = nc.dram_tensor("dbg", [128, 20 * 64], F32, kind="ExternalOutput").ap()
        P.dma("sp", lambda e: e.dma_start(out=dbg_d, in_=small[:].rearrange("p a n -> p (a n)")), "dbgc",
              reads=[("sm", i) for i in range(20)])
        fch.append("dbgc")
    P.emit(nc, fch)
    st.close()
    return nc


def _fm(v):
    v = np.asarray(v, np.float32)
    lead = v.shape[:-1]
    a = v.reshape(lead + (KC, 128))
    a = np.moveaxis(a, -1, 0)
    return np.ascontiguousarray(a)


def _consts():
    c = np.zeros((128, 6, 128), np.float32)
    i = np.arange(128)
    c[:, 0, :] = np.eye(128, dtype=np.float32)
    tri = (i[:, None] <= i[None, :]).astype(np.float32)
    c[:, 1, :] = tri
    c[:, 2, :] = -tri
    c[:, 3, :] = 1.0
    c[:, 4, :] = 1.0 / 1024.0
    c[:, 5, 0:32] = ((i[:, None] % 32) <= np.arange(32)[None, :]).astype(np.float32)
    return c


def make_in_maps(inputs, NT, cores):
    I = {k: np.asarray(v) for k, v in inputs.items()}
    shared = {
        "ada_w": np.ascontiguousarray(I["ada_w"], np.float32),
        "ada_b": np.ascontiguousarray(np.moveaxis(I["ada_b"].reshape(DEPTH, 48, 128), -1, 0), np.float32),
        "norm1_w": _fm(I["norm1_w"]),
        "norm2_w": _fm(I["norm2_w"]),
        "final_norm_w": _fm(I["final_norm_w"]),
        "m_gate_bias": np.ascontiguousarray(np.broadcast_to(
            np.concatenate([I["m_i_bias"], I["m_f_bias"]], axis=-1)[None], (128, 2, 16)), np.float32),
        "m_conv_w": np.ascontiguousarray(np.transpose(_fm(I["m_conv_w"]), (0, 1, 3, 2)), np.float32),
        "m_conv_b": _fm(I["m_conv_b"]),
        "m_norm_w": _fm(I["m_norm_w"]),
        "h_norm_w": _fm(I["h_norm_w"]),
        "h_lb_logits": _fm(I["h_lb_logits"]),
        "moe_router": np.ascontiguousarray(
            np.transpose(I["moe_router"].reshape(2, KC, 128, NE), (2, 0, 1, 3)), np.float32),
        "consts": _consts(),
    }
    for n in ("m_w_in", "m_w_out", "h_w_in", "h_w_out", "ffn_w_gate", "ffn_w_up", "ffn_w_down",
              "moe_w_gate", "moe_w_up", "moe_w_down"):
        shared[n] = np.ascontiguousarray(I[n], np.float32)
    maps = []
    for b in cores:
        m = dict(shared)
        m["x"] = np.ascontiguousarray(I["x"][b, :NT * TS, :], np.float32)
        m["c"] = _fm(I["c"][b])
        maps.append(m)
    return maps


_NC_CACHE = {}


def run(inputs, NT=SEQ // TS, NL=DEPTH, final=True, cores=tuple(range(8)), trace=False, half="full"):
    key = (NT, NL, final, half)
    if key not in _NC_CACHE:
        _NC_CACHE[key] = build(NT, NL, final, half)
    nc = _NC_CACHE[key]
    maps = make_in_maps(inputs, NT, cores)
    res = run_bass_kernel_spmd(nc, maps, core_ids=list(range(len(cores))), trace=trace)
    out = np.stack([np.asarray(r["out"], np.float32) for r in res.results], axis=0)
    if os.environ.get("KDUMP"):
        np.save(os.environ["KDUMP"], np.asarray(res.results[0]["dbg"]))
    return out, res


def kernel(**inputs):
    out, _ = run(inputs)
    return out.astype(np.float32)
```

```python
import os
import numpy as np
from contextlib import ExitStack
import concourse.bass as bass
import concourse.mybir as mybir
from concourse.bass_utils import run_bass_kernel_spmd

F32 = mybir.dt.float32
BF16 = mybir.dt.bfloat16
AF = mybir.ActivationFunctionType
ALU = mybir.AluOpType
AX = mybir.AxisListType

D = 1024
KC = 8
SEQ = 4096
TS = 512
NS = TS // 128
DEPTH = 4
M_PROJ = 3088
HG_PROJ = 4096
D_FF = 2816
NE = 8
D_FFE = 1408
EPS = 1e-6
DBG = float(os.environ.get("KDBG", "99"))
NSLOT = 6
NCAST = 3


class Buf:
    __slots__ = ("ap", "keys")

    def __init__(self, ap, keys):
        self.ap = ap
        self.keys = tuple(keys)


class Prog:
    ENG = ("pe", "act", "dve", "pool", "sp")

    def __init__(self):
        self.ops = {e: [] for e in self.ENG}
        self.tick = {e: 0 for e in self.ENG}
        self.chan = {}
        self.lastw = {}
        self.readers = {}
        self.seen = {e: {} for e in self.ENG}

    def _deps(self, eng, reads, writes):
        deps = {}

        def add(ev):
            if ev is None:
                return
            s, v = ev
            if deps.get(s, 0) < v:
                deps[s] = v

        for k in reads:
            add(self.lastw.get(k))
            if k[0] == "ps":
                for s, v in self.readers.get(k, {}).items():
                    if s != eng:
                        add((s, v))
        for k in writes:
            add(self.lastw.get(k))
            for s, v in self.readers.get(k, {}).items():
                add((s, v))
        waits = []
        seen = self.seen[eng]
        for s, v in deps.items():
            if s == "pe" and eng == "pe":
                continue
            if seen.get(s, 0) >= v:
                continue
            seen[s] = v
            waits.append((s, v))
        return waits

    def _commit(self, ev, reads, writes):
        s, v = ev
        for k in reads:
            self.readers.setdefault(k, {})[s] = v
        for k in writes:
            self.lastw[k] = ev
            self.readers[k] = {}

    def op(self, eng, fn, reads=(), writes=()):
        waits = self._deps(eng, reads, writes)
        self.tick[eng] += 1
        ev = (eng, self.tick[eng])
        self.ops[eng].append((fn, waits, ev, False))
        self._commit(ev, reads, writes)

    def dma(self, eng, fn, chan, reads=(), writes=()):
        writes = tuple(writes) + (("chan", chan),)
        waits = self._deps(eng, reads, writes)
        self.chan[chan] = self.chan.get(chan, 0) + 16
        ev = ("d:" + chan, self.chan[chan])
        self.ops[eng].append((fn, waits, ev, True))
        self._commit(ev, reads, writes)

    def emit(self, nc, final_chans):
        with ExitStack() as st:
            sems = {}
            for n in list(self.ENG) + ["d:" + c for c in self.chan]:
                sems[n] = st.enter_context(nc.semaphore("s_" + n.replace(":", "_")))
            block = st.enter_context(nc.Block())

            def run(name, e):
                for fn, waits, ev, isdma in self.ops[name]:
                    for s, v in waits:
                        e.wait_ge(sems[s], v)
                    ins = fn(e)
                    ins.then_inc(sems[ev[0]], 16 if isdma else 1)
                if name == "sp":
                    for c in final_chans:
                        e.wait_ge(sems["d:" + c], self.chan[c])

            @block.tensor
            def _(e):
                run("pe", e)

            @block.scalar
            def _(e):
                run("act", e)

            @block.vector
            def _(e):
                run("dve", e)

            @block.gpsimd
            def _(e):
                run("pool", e)

            @block.sync
            def _(e):
                run("sp", e)


def build(NT=8, NL=DEPTH, final=True, half="full"):
    nc = bass.Bass("TRN2", target_bir_lowering=False)
    P = Prog()
    st = ExitStack()

    def din(name, shape, dt=F32):
        return nc.dram_tensor(name, list(shape), dt, kind="ExternalInput").ap()

    x_d = din("x", [NT * TS, D])
    out_d = nc.dram_tensor("out", [NT * TS, D], F32, kind="ExternalOutput").ap()
    c_d = din("c", [128, KC])
    ada_w_d = din("ada_w", [DEPTH, D, 6 * D])
    ada_b_d = din("ada_b", [128, DEPTH, 48])
    n1_d = din("norm1_w", [128, DEPTH, KC])
    n2_d = din("norm2_w", [128, DEPTH, KC])
    fn_d = din("final_norm_w", [128, KC])
    gb_d = din("m_gate_bias", [128, 2, 16])
    cw_d = din("m_conv_w", [128, 2, KC, 4])
    cb_d = din("m_conv_b", [128, 2, KC])
    mnw_d = din("m_norm_w", [128, 2, KC])
    hnw_d = din("h_norm_w", [128, 2, KC])
    lb_d = din("h_lb_logits", [128, 2, KC])
    rt_d = din("moe_router", [128, 2, KC, NE])
    cst_d = din("consts", [128, 6, 128])
    wshapes = {
        "m_w_in": [2, D, M_PROJ], "m_w_out": [2, D, D], "h_w_in": [2, D, HG_PROJ], "h_w_out": [2, D, D],
        "ffn_w_gate": [2, D, D_FF], "ffn_w_up": [2, D, D_FF], "ffn_w_down": [2, D_FF, D],
        "moe_w_gate": [2, NE, D, D_FFE], "moe_w_up": [2, NE, D, D_FFE], "moe_w_down": [2, NE, D_FFE, D],
    }
    wsrc = {}
    wdst = {}
    for n, shp in wshapes.items():
        wsrc[n] = din(n, shp)
        wdst[n] = nc.dram_tensor(n + "_b", list(shp), BF16, kind="Internal").ap()

    def sb(name, shape, dt=F32):
        return st.enter_context(nc.sbuf_tensor(name, list(shape), dt))

    ps = st.enter_context(nc.psum_tensor("ps", [128, 8, 512], F32))

    def bank(i):
        return Buf(ps[:, i, :], [("ps", i)])

    xT = sb("xT", [128, KC, TS])
    tmp = sb("tmp", [128, KC, TS])
    hnT = sb("hnT", [128, KC, TS], BF16)
    hhnT = sb("hhnT", [128, KC, TS], BF16)
    sq = hhnT
    rstd = sb("rstd", [128, TS])
    ring = sb("ring", [128, NSLOT, 4096], BF16)
    qkT = sb("qkT", [128, KC, TS], BF16)
    khT = sb("khT", [128, KC, TS], BF16)
    gT = sb("gT", [128, KC, TS], BF16)
    vt = sb("vt", [128, NS, 8, 129], BF16)
    cv = sb("cv", [128, 2, TS + 3])
    acc = sb("acc", [128, 2, TS])
    ktm = sb("ktm", [128, 2, D], BF16)
    Sm = sb("Sm", [128, 2, 8, 128], BF16)
    hh = sb("hh", [128, 2, D], BF16)
    ft4 = sb("ft4", [128, 7, TS])
    sqn = ft4[:, 0:2, :].rearrange("p a n -> p (a n)")
    small = sb("small", [128, 20, 64])
    Cst = sb("Cst", [128, 2, 4, 129])
    Cbf = sb("Cbf", [128, 2, 2, 4, 129], BF16)
    Sst = sb("Sst", [128, 2, 8, 128])
    Sbf = sb("Sbf", [128, 4, 8, 128], BF16)
    amT = sb("amT", [128, 2, 8, 128], BF16)
    hist = sb("hist", [128, 2, KC, 3])
    cst = sb("cst", [128, 6, 128])
    cstb = sb("cstb", [128, 2, 128], BF16)
    cond = sb("cond", [128, KC])
    epsc = sb("epsc", [128, 1])
    l8c = sb("l8c", [128, 1])
    onec = sb("onec", [128, 1])
    modv = sb("modv", [128, DEPTH, 48])
    adabias = sb("adabias", [128, DEPTH, 48])
    n1 = sb("n1", [128, DEPTH, KC])
    n2 = sb("n2", [128, DEPTH, KC])
    fnw = sb("fnw", [128, KC])
    A1 = sb("A1", [128, DEPTH, KC])
    A2 = sb("A2", [128, DEPTH, KC])
    gbias = sb("gbias", [128, 2, 16])
    cw = sb("cw", [128, 2, KC, 4])
    cb = sb("cb", [128, 2, KC])
    mnw = sb("mnw", [128, 2, KC])
    hnw = sb("hnw", [128, 2, KC])
    lbl = sb("lbl", [128, 2, KC])
    lbv = sb("lbv", [128, 2, KC])
    omlb = sb("omlb", [128, 2, KC])
    rt = sb("rt", [128, 2, KC, NE])
    gbc = tmp

    def mm(out, lhsT, rhs, start=True, stop=True):
        o, l, r = out.ap, lhsT.ap, rhs.ap
        P.op("pe", lambda e: e.matmul(o, l, r, start=start, stop=stop),
             reads=lhsT.keys + rhs.keys, writes=out.keys)

    def tr(out, in_, ident):
        o, i, d = out.ap, in_.ap, ident.ap
        P.op("pe", lambda e: e.transpose(o, i, d), reads=in_.keys + ident.keys, writes=out.keys)

    def act(out, in_, func, scale=1.0, bias=0.0, extra=()):
        o, i = out.ap, in_.ap
        P.op("act", lambda e: e.activation(out=o, in_=i, func=func, scale=scale, bias=bias),
             reads=in_.keys + tuple(extra), writes=out.keys)

    def tt(out, a, b, op, eng="dve"):
        o, x0, x1 = out.ap, a.ap, b.ap
        P.op(eng, lambda e: e.tensor_tensor(out=o, in0=x0, in1=x1, op=op),
             reads=a.keys + b.keys, writes=out.keys)

    def tsc(out, a, s1, s2, op0, op1=None, extra=(), eng="dve"):
        o, x0 = out.ap, a.ap
        if op1 is None:
            P.op(eng, lambda e: e.tensor_scalar(out=o, in0=x0, scalar1=s1, scalar2=None, op0=op0),
                 reads=a.keys + tuple(extra), writes=out.keys)
        else:
            P.op(eng, lambda e: e.tensor_scalar(out=o, in0=x0, scalar1=s1, scalar2=s2, op0=op0, op1=op1),
                 reads=a.keys + tuple(extra), writes=out.keys)

    def stt(out, a, s, b, op0, op1, extra=(), eng="dve"):
        o, x0, x1 = out.ap, a.ap, b.ap
        P.op(eng, lambda e: e.scalar_tensor_tensor(out=o, in0=x0, scalar=s, in1=x1, op0=op0, op1=op1),
             reads=a.keys + b.keys + tuple(extra), writes=out.keys)

    def rsqrt_eps(out, in_, scale=1.0):
        act(out, in_, AF.Sqrt, scale=scale, bias=epsc[:out.ap.shape[0], 0:1], extra=[("epsc",)])
        o = out.ap
        P.op("dve", lambda e: e.reciprocal(out=o, in_=o), reads=out.keys, writes=out.keys)

    def cp(out, in_, eng="dve"):
        o, i = out.ap, in_.ap
        if eng == "act":
            P.op("act", lambda e: e.activation(out=o, in_=i, func=AF.Copy), reads=in_.keys, writes=out.keys)
        else:
            P.op(eng, lambda e: e.tensor_copy(out=o, in_=i), reads=in_.keys, writes=out.keys)

    def red(out, in_, op, eng="dve"):
        o, i = out.ap, in_.ap
        P.op(eng, lambda e: e.tensor_reduce(out=o, in_=i, axis=AX.X, op=op), reads=in_.keys, writes=out.keys)

    def memset(out, val, eng="dve"):
        o = out.ap
        P.op(eng, lambda e: e.memset(o, val), writes=out.keys)

    def load(dst, src_ap, chan, eng="sp", reads=()):
        o = dst.ap
        P.dma(eng, lambda e: e.dma_start(out=o, in_=src_ap), chan, reads=reads, writes=dst.keys)

    ring_ctr = [0]

    def ring_load(src_ap, shape, wkey):
        i = ring_ctr[0] % NSLOT
        ring_ctr[0] += 1
        n = int(np.prod(shape[1:]))
        if len(shape) == 3:
            view = ring[:, i, 0:n].rearrange("p (k n) -> p k n", k=shape[1])
        else:
            view = ring[:, i, 0:n]
        b = Buf(view, [("ring", i)])
        load(b, src_ap, "ring%d" % i, reads=[wkey])
        return b

    def wpiece(name, idx, c0, w):
        a = wdst[name]
        for i in idx:
            a = a[i]
        return a[:, c0:c0 + w].rearrange("(k p) n -> p k n", p=128)

    cast_ctr = [0]

    def cast(name, idx):
        s, d_ = wsrc[name], wdst[name]
        for i in idx:
            s, d_ = s[i], d_[i]
        sf = s.rearrange("r c -> (r c)").rearrange("(a b) -> a b", b=2048)
        df = d_.rearrange("r c -> (r c)").rearrange("(a b) -> a b", b=2048)
        ch = "cast%d" % (cast_ctr[0] % NCAST)
        cast_ctr[0] += 1
        P.dma("pool", lambda e: e.dma_start(out=df, in_=sf), ch, writes=[("wb", name) + tuple(idx)])

    for j in range(2):
        if 2 * j < NL:
            for n in ("m_w_in", "m_w_out", "ffn_w_gate", "ffn_w_up", "ffn_w_down"):
                cast(n, (j,))
        if 2 * j + 1 < NL:
            for n in ("h_w_in", "h_w_out"):
                cast(n, (j,))
            for e_ in range(NE):
                for n in ("moe_w_gate", "moe_w_up", "moe_w_down"):
                    cast(n, (j, e_))

    def K(name, *idx):
        return [(name,) + tuple(idx)] if idx else [(name,)]

    def cload(tile, src, name):
        load(Buf(tile[:], K(name)), src, "const")

    cload(cst, cst_d, "cst")
    cload(cond, c_d, "cond")
    cload(adabias, ada_b_d, "adabias")
    cload(n1, n1_d, "n1")
    cload(n2, n2_d, "n2")
    cload(fnw, fn_d, "fnw")
    cload(gbias, gb_d, "gbias")
    cload(cw, cw_d, "cw")
    cload(cb, cb_d, "cb")
    cload(mnw, mnw_d, "mnw")
    cload(hnw, hnw_d, "hnw")
    cload(lbl, lb_d, "lbl")
    cload(rt, rt_d, "rt")
    ident32 = Buf(cst[:, 0, :], K("cst"))
    mask32 = Buf(cst[:, 1, :], K("cst"))
    tripos = Buf(cst[:, 1, :], K("cst"))
    onespos = Buf(cst[:, 3, :], K("cst"))
    mask64 = Buf(cst[:, 5, :], K("cst"))
    cp(Buf(cstb[:, 0, :], K("cstb")), Buf(cst[:, 0, :], K("cst")))
    cp(Buf(cstb[:, 1, :], K("cstb")), Buf(cst[:, 4, :], K("cst")))
    identb = Buf(cstb[:, 0, :], K("cstb"))
    onesdiv = Buf(cstb[:, 1, :], K("cstb"))

    memset(Buf(epsc[:], [("epsc",)]), EPS)
    memset(Buf(l8c[:], [("l8c",)]), float(np.log(0.125)))
    memset(Buf(onec[:], [("onec",)]), 1.0)
    act(Buf(cond[:], K("cond")), Buf(cond[:], K("cond")), AF.Silu)
    memset(Buf(lbv[:, 0, :], K("lbv")), 0.0)
    tt(Buf(lbv[:, 1, :], K("lbv")), Buf(lbl[:, 1, :], K("lbl")), Buf(lbl[:, 0, :], K("lbl")), ALU.subtract)
    act(Buf(lbv[:, 1, :], K("lbv")), Buf(lbv[:, 1, :], K("lbv")), AF.Sigmoid)
    tsc(Buf(omlb[:], K("omlb")), Buf(lbv[:], K("lbv")), -1.0, 1.0, ALU.mult, ALU.add)
    memset(Buf(Cst[:], [("Cst", 0), ("Cst", 1)]), 0.0)
    memset(Buf(Cbf[:], [("Cbf", a_, b_) for a_ in range(2) for b_ in range(2)]), 0.0)
    memset(Buf(Sst[:], [("Sst", 0), ("Sst", 1)]), 0.0)
    memset(Buf(hist[:], K("hist")), 0.0)
    memset(Buf(small[:], [("sm", i_) for i_ in range(20)]), 0.0)
    memset(Buf(amT[:], [("amT", 0), ("amT", 1)]), 0.0)
    memset(Buf(vt[:], [("vt", s_) for s_ in range(NS)]), 1.0)
    memset(Buf(ft4[:, 6, :], K("ft4", 6)), 1.0)
    memset(Buf(ft4[:, 6, 0:TS:32], K("ft4", 6)), 0.0)

    for l in range(NL):
        for g in range(12):
            bslot = g % 3
            adab_v = ring[:, 2 * bslot:2 * bslot + 2, :].rearrange("p a n -> p (a n)").bitcast(F32).rearrange(
                "p (k n) -> p k n", k=KC)
            adk = [("ring", 2 * bslot), ("ring", 2 * bslot + 1)]
            dst = Buf(adab_v, adk)
            load(dst, ada_w_d[l, :, g * 512:(g + 1) * 512].rearrange("(k p) n -> p k n", p=128), "ada%d" % bslot)
            for nci in range(4):
                col = g * 4 + nci
                o = Buf(ps[:, 7, col:col + 1], [("ps", 7)])
                for k in range(KC):
                    mm(o, Buf(adab_v[:, k, nci * 128:(nci + 1) * 128], adk),
                       Buf(cond[:, k:k + 1], K("cond")), start=(k == 0), stop=(k == KC - 1))
        tt(Buf(modv[:, l, :], K("modv", l)), Buf(ps[:, 7, 0:48], [("ps", 7)]),
           Buf(adabias[:, l, :], K("adabias")), ALU.add)
        stt(Buf(A1[:, l, :], K("A1", l)), Buf(modv[:, l, 8:16], K("modv", l)), 1.0, Buf(n1[:, l, :], K("n1")),
            ALU.add, ALU.mult)
        stt(Buf(A2[:, l, :], K("A2", l)), Buf(modv[:, l, 32:40], K("modv", l)), 1.0, Buf(n2[:, l, :], K("n2")),
            ALU.add, ALU.mult)

    def shift1(l, c):
        return modv[:, l, 0 + c:1 + c]

    def gate1(l, c):
        return modv[:, l, 16 + c:17 + c]

    def shift2(l, c):
        return modv[:, l, 24 + c:25 + c]

    def gate2(l, c):
        return modv[:, l, 40 + c:41 + c]

    xk = [("xT", c) for c in range(KC)]
    hnk = [("hnT", c) for c in range(KC)]
    tmpk = [("tmp", c) for c in range(KC)]

    def rms_stats():
        for k in range(KC):
            if k % 2 == 0:
                act(Buf(sq[:, k, :], [("hhnT", k)]), Buf(xT[:, k, :], [("xT", k)]), AF.Square)
            else:
                tt(Buf(sq[:, k, :], [("hhnT", k)]), Buf(xT[:, k, :], [("xT", k)]), Buf(xT[:, k, :], [("xT", k)]), ALU.mult)
            mm(bank(0), onesdiv, Buf(sq[:, k, :], [("hhnT", k)]), start=(k == 0), stop=(k == KC - 1))
        rsqrt_eps(Buf(rstd[:], K("rstd")), bank(0))

    def norm_mod(l, which, keep32):
        rms_stats()
        A = A1 if which == 1 else A2
        for c in range(KC):
            tt(Buf(tmp[:, c, :], [("tmp", c)]), Buf(xT[:, c, :], [("xT", c)]), Buf(rstd[:], K("rstd")), ALU.mult)
            sh = shift1(l, c) if which == 1 else shift2(l, c)
            extra = K("A1" if which == 1 else "A2", l) + K("modv", l)
            if keep32:
                act(Buf(tmp[:, c, :], [("tmp", c)]), Buf(tmp[:, c, :], [("tmp", c)]), AF.Identity,
                    scale=A[:, l, c:c + 1], bias=sh, extra=extra)
                cp(Buf(hnT[:, c, :], [("hnT", c)]), Buf(tmp[:, c, :], [("tmp", c)]), eng="pool")
            else:
                act(Buf(hnT[:, c, :], [("hnT", c)]), Buf(tmp[:, c, :], [("tmp", c)]), AF.Identity,
                    scale=A[:, l, c:c + 1], bias=sh, extra=extra)

    def resid_add(dc, psb, gate_ap, l):
        stt(Buf(xT[:, dc, :], [("xT", dc)]), psb, gate_ap, Buf(xT[:, dc, :], [("xT", dc)]), ALU.mult, ALU.add,
            extra=K("modv", l))

    pbc = [0]

    def next_bank(lo, n):
        i = lo + pbc[0] % n
        pbc[0] += 1
        return i

    def proj_fm(slot, ncols, consume):
        for nci in range(ncols // 128):
            b = bank(next_bank(1, 6))
            for k in range(KC):
                mm(b, Buf(slot.ap[:, k, nci * 128:(nci + 1) * 128], slot.keys), Buf(hnT[:, k, :], [("hnT", k)]),
                   start=(k == 0), stop=(k == KC - 1))
            consume(nci, b)

    def out_proj(name, j, l):
        for g in range(2):
            slot = ring_load(wpiece(name, (j,), g * 512, 512), [128, KC, 512], ("wb", name, j))
            for nci in range(4):
                dc = g * 4 + nci
                b = bank(next_bank(1, 6))
                for k in range(KC):
                    mm(b, Buf(slot.ap[:, k, nci * 128:(nci + 1) * 128], slot.keys),
                       Buf(hhnT[:, k, :], [("hhnT", k)]), start=(k == 0), stop=(k == KC - 1))
                resid_add(dc, b, gate1(l, dc), l)

    def run_skewed(body):
        gens = [body(s_) for s_ in range(NS)]
        next(gens[0])
        for s_ in range(NS):
            next(gens[s_])
            if s_ + 1 < NS:
                next(gens[s_ + 1])
            for _ in gens[s_]:
                pass

    def mlstm(l, ti):
        j = l // 2
        wk = ("wb", "m_w_in", j)
        slot = ring_load(wpiece("m_w_in", (j,), 3072, 16), [128, KC, 16], wk)
        gps = Buf(ps[:, 7, 0:NS * 16].rearrange("p (s n) -> p s n", s=NS), [("ps", 7)])
        for s in range(NS):
            o = Buf(ps[:, 7, s * 16:(s + 1) * 16], [("ps", 7)])
            for k in range(KC):
                mm(o, Buf(hnT[:, k, s * 128:(s + 1) * 128], [("hnT", k)]), Buf(slot.ap[:, k, :], slot.keys),
                   start=(k == 0), stop=(k == KC - 1))
        G = small[:, 0, 0:NS * 16].rearrange("p (s n) -> p s n", s=NS)
        tt(Buf(G, K("sm", 0)), gps, Buf(gbias[:, j, :].unsqueeze(1).to_broadcast([128, NS, 16]), K("gbias")),
           ALU.add)
        if DBG <= 2.2:
            return
        th = small[:, 1, 0:NS * 8].rearrange("p (s n) -> p s n", s=NS)
        act(Buf(th, K("sm", 1)), Buf(G[:, :, 0:8], K("sm", 0)), AF.Tanh, scale=1.0 / 15.0)
        spv = small[:, 2, 0:NS * 8].rearrange("p (s n) -> p s n", s=NS)
        act(Buf(spv, K("sm", 2)), Buf(G[:, :, 8:16], K("sm", 0)), AF.Exp, scale=-1.0)
        act(Buf(spv, K("sm", 2)), Buf(spv, K("sm", 2)), AF.Ln, bias=1.0)
        if DBG <= 2.4:
            return
        bps = ps[:, 7, 64:64 + NS * 16].rearrange("p (s n) -> p s n", s=NS)
        for s in range(NS):
            mm(Buf(ps[:, 7, 64 + s * 16:64 + s * 16 + 8], [("ps", 7)]), tripos, Buf(spv[:, s, :], K("sm", 2)))
            mm(Buf(ps[:, 7, 64 + s * 16 + 8:64 + s * 16 + 16], [("ps", 7)]), onespos, Buf(spv[:, s, :], K("sm", 2)))
        if DBG <= 2.6:
            return
        bps_b = Buf(bps, [("ps", 7)])
        eb = small[:, 3, 0:NS * 16].rearrange("p (s n) -> p s n", s=NS)
        act(Buf(eb, K("sm", 3)), bps_b, AF.Exp, scale=-1.0)
        if DBG <= 2.7:
            return
        wv = small[:, 4, 0:NS * 8].rearrange("p (s n) -> p s n", s=NS)
        bsb = small[:, 11, 0:NS * 16].rearrange("p (s n) -> p s n", s=NS)
        cp(Buf(bsb, K("sm", 11)), bps_b)
        stt(Buf(wv, K("sm", 4)), Buf(th, K("sm", 1)), 15.0, Buf(bsb[:, :, 0:8], K("sm", 11)), ALU.mult, ALU.add)
        act(Buf(wv, K("sm", 4)), Buf(wv, K("sm", 4)), AF.Exp, bias=l8c[:, 0:1], extra=[("l8c",)])
        if DBG <= 2.8:
            return
        EL = small[:, 5, 0:NS * 4].rearrange("p (s n) -> p s n", s=NS)
        cp(Buf(EL[0:64], K("sm", 5)), Buf(eb[0:64, :, 8:16:2], K("sm", 3)))
        cp(Buf(EL[64:128], K("sm", 5)), Buf(eb[64:128, :, 9:16:2], K("sm", 3)))

        if DBG <= 3:
            return
        pend_silu = []
        for qi in range(2):
            slot = ring_load(wpiece("m_w_in", (j,), qi * 512, 512), [128, KC, 512], wk)

            def consume(nci, b, qi=qi):
                c = qi * 4 + nci
                cb_ = c % 2
                cvb = Buf(cv[:, cb_, :], [("cv", cb_)])
                act(Buf(cv[:, cb_, 3:3 + TS], [("cv", cb_)]), b, AF.Copy)
                while pend_silu:
                    pend_silu.pop(0)()
                cp(Buf(cv[:, cb_, 0:3], [("cv", cb_)]), Buf(hist[:, j, c, :], K("hist")), eng="pool")
                ab = Buf(acc[:, cb_, :], [("acc", cb_)])
                tsc(ab, Buf(cv[:, cb_, 0:TS], cvb.keys), cw[:, j, c, 0:1], cb[:, j, c:c + 1], ALU.mult, ALU.add,
                    extra=K("cw") + K("cb"))
                for tap in range(1, 4):
                    stt(ab, Buf(cv[:, cb_, tap:tap + TS], cvb.keys), cw[:, j, c, tap:tap + 1], ab, ALU.mult, ALU.add,
                        extra=K("cw"))
                cp(Buf(hist[:, j, c, :], K("hist")), Buf(cv[:, cb_, TS:TS + 3], cvb.keys), eng="pool")
                pend_silu.append(lambda c=c, ab=ab: act(Buf(qkT[:, c, :], [("qk", c)]), ab, AF.Silu))

            proj_fm(slot, 512, consume)
        while pend_silu:
            pend_silu.pop(0)()
        if DBG <= 4:
            return
        for oi in range(2):
            slot = ring_load(wpiece("m_w_in", (j,), 2048 + oi * 512, 512), [128, KC, 512], wk)

            def consume(nci, b, oi=oi):
                c = oi * 4 + nci
                act(Buf(gT[:, c, :], [("gT", c)]), b, AF.Sigmoid)
                tsc(Buf(gT[:, c, :], [("gT", c)]), Buf(gT[:, c, :], [("gT", c)]), mnw[:, j, c:c + 1], None, ALU.mult,
                    extra=K("mnw"))

            proj_fm(slot, 512, consume)
        for vi in range(2):
            slot = ring_load(wpiece("m_w_in", (j,), 1024 + vi * 512, 512), [128, KC, 512], wk)
            for s in range(NS):
                b = bank(next_bank(1, 6))
                for k in range(KC):
                    mm(b, Buf(hnT[:, k, s * 128:(s + 1) * 128], [("hnT", k)]), Buf(slot.ap[:, k, :], slot.keys),
                       start=(k == 0), stop=(k == KC - 1))
                tt(Buf(vt[:, s, vi * 4:(vi + 1) * 4, 0:128], [("vt", s)]),
                   Buf(b.ap.rearrange("p (h n) -> p h n", h=4), b.keys),
                   Buf(wv[:, s, vi * 4:(vi + 1) * 4].unsqueeze(2).to_broadcast([128, 4, 128]), K("sm", 4)), ALU.mult)
        for s in range(NS):
            cp(Buf(vt[:, s, :, 128], [("vt", s)]), Buf(wv[:, s, :], K("sm", 4)))

        if DBG <= 5:
            return
        def body(s):
            gi = ti * NS + s
            pb_ = gi % 2
            t0 = s * 128
            ktp = Buf(ps[:, 0, :].bitcast(BF16)[:, 0:512], [("ps", 0)])
            for c in range(4):
                tr(Buf(ps[:, 0, :].bitcast(BF16)[:, c * 128:(c + 1) * 128], [("ps", 0)]),
                   Buf(qkT[:, 4 + c, t0:t0 + 128], [("qk", 4 + c)]), identb)
            kb = Buf(ktm[:, pb_, 0:512], [("ktm", pb_)])
            cp(kb, ktp, eng="act")
            stb = Buf(ps[:, 1:3, :].rearrange("p b n -> p (b n)"), [("ps", 1), ("ps", 2)])
            for h in range(8):
                c, po = h // 2, (h % 2) * 64
                mm(Buf(ps[:, 1 + h % 2, (h // 2) * 128:(h // 2 + 1) * 128], stb.keys),
                   Buf(qkT[po:po + 64, 4 + c, t0:t0 + 128], [("qk", 4 + c)]),
                   Buf(qkT[po:po + 64, c, t0:t0 + 128], [("qk", c)]))
            smb = Buf(Sm[:, pb_], [("Sm", pb_)])
            tt(Buf(Sm[:, pb_].rearrange("p (pr par) n -> p par pr n", par=2), smb.keys),
               Buf(ps[:, 1:3, :].rearrange("p b (q n) -> p b q n", q=4), stb.keys),
               Buf(mask32.ap.unsqueeze(1).unsqueeze(1).to_broadcast([128, 2, 4, 128]), mask32.keys), ALU.mult)
            dck = [("ps", 6), ("ps", 7)]
            for h in range(8):
                pr, po = h // 2, (h % 2) * 64
                mm(Buf(ps[po:po + 64, 6 + pr // 2, (pr % 2) * 256:(pr % 2) * 256 + 129], dck),
                   Buf(ktm[:, pb_, h * 64:(h + 1) * 64], kb.keys), Buf(vt[:, s, h, :], [("vt", s)]))
            cprev = gi % 2
            dcv = Buf(ps[:, 6:8, :].rearrange("p b (q n) -> p (b q) n", q=2)[:, :, 0:129], dck)
            cs = Buf(Cst[:, j], K("Cst", j))
            tt(cs, cs, dcv, ALU.add)
            tt(cs, cs, Buf(EL[:, s, :].unsqueeze(2).to_broadcast([128, 4, 129]), K("sm", 5)), ALU.mult)
            cp(Buf(Cbf[:, j, 1 - cprev], [("Cbf", j, 1 - cprev)]), cs, eng="pool")
            yield
            numk = [("ps", 3), ("ps", 4)]
            denk = [("ps", 5)]
            cprev = gi % 2
            for h in range(8):
                c, po, pr = h // 2, (h % 2) * 64, h // 2
                o = Buf(ps[:, 3 + h // 4, (h % 4) * 128:(h % 4 + 1) * 128], numk)
                mm(o, Buf(Sm[:, pb_, h, :], smb.keys), Buf(vt[:, s, h, 0:128], [("vt", s)]), start=True, stop=False)
                mm(o, Buf(qkT[po:po + 64, c, t0:t0 + 128], [("qk", c)]),
                   Buf(Cbf[po:po + 64, j, cprev, pr, 0:128], [("Cbf", j, cprev)]), start=False, stop=True)
            for h in range(8):
                c, po, pr = h // 2, (h % 2) * 64, h // 2
                o = Buf(ps[:, 5, h:h + 1], denk)
                mm(o, Buf(Sm[:, pb_, h, :], smb.keys), Buf(vt[:, s, h, 128:129], [("vt", s)]), start=True, stop=False)
                mm(o, Buf(qkT[po:po + 64, c, t0:t0 + 128], [("qk", c)]),
                   Buf(Cbf[po:po + 64, j, cprev, pr, 128:129], [("Cbf", j, cprev)]), start=False, stop=True)
            numv = Buf(ps[:, 3:5, :].rearrange("p b (q n) -> p (b q) n", q=4), numk)
            den = Buf(ps[:, 5, 0:8], denk)
            ebs = Buf(eb[:, s, 0:8], K("sm", 3))
            r0 = Buf(small[:, 6, 0:8], K("sm", 6))
            r1 = Buf(small[:, 7, 0:8], K("sm", 7))
            r2 = Buf(small[:, 8, 0:8], K("sm", 8))
            r3 = Buf(small[:, 9, 0:8], K("sm", 9))
            act(r0, den, AF.Abs)
            tt(r0, r0, ebs, ALU.mult)
            tsc(r0, r0, 1.0, None, ALU.max)
            P.op("dve", lambda e, o=r1.ap, i=r0.ap: e.reciprocal(out=o, in_=i), reads=r0.keys, writes=r1.keys)
            tt(r1, r1, ebs, ALU.mult)
            sqb = Buf(sqn.rearrange("p (h n) -> p h n", h=8), [("ft4", 0), ("ft4", 1)])
            act(sqb, numv, AF.Square)
            red(r2, sqb, ALU.add)
            tt(r3, r1, r1, ALU.mult)
            tt(r2, r2, r3, ALU.mult)
            rsqrt_eps(r2, r2, scale=1.0 / 128.0)
            tt(r2, r2, r1, ALU.mult)
            hb = Buf(hh[:, pb_, :], [("hh", pb_)])
            tt(Buf(hh[:, pb_, :].rearrange("p (h n) -> p h n", h=8), hb.keys), numv,
               Buf(r2.ap.unsqueeze(2).to_broadcast([128, 8, 128]), r2.keys), ALU.mult)
            yield
            tpb = Buf(ps[:, 0, :].bitcast(BF16), [("ps", 0)])
            for c in range(8):
                tr(Buf(ps[:, 0, :].bitcast(BF16)[:, c * 128:(c + 1) * 128], [("ps", 0)]),
                   Buf(hh[:, pb_, c * 128:(c + 1) * 128], hb.keys), identb)
            tt(Buf(hhnT[:, :, t0:t0 + 128], [("hhnT", c) for c in range(8)]),
               Buf(tpb.ap.rearrange("p (c n) -> p c n", c=8), tpb.keys),
               Buf(gT[:, :, t0:t0 + 128], [("gT", c) for c in range(8)]), ALU.mult)
        run_skewed(body)
        out_proj("m_w_out", j, l)

    def hgrn(l, ti):
        j = l // 2
        wk = ("wb", "h_w_in", j)
        CH = 32
        NCH = TS // CH
        CPS = 128 // CH

        def sm2(i):
            return small[:, i:i + 2, :].rearrange("p a n -> p (a n)").rearrange("p (h c) -> p h c", h=8)

        Bref, BLv, eref, eL, eLR = sm2(10), sm2(12), sm2(14), sm2(16), sm2(18)
        kB, kBL, kER, kEL, kELR = (K("sm", 10) + K("sm", 11), K("sm", 12) + K("sm", 13), K("sm", 14) + K("sm", 15),
                                   K("sm", 16) + K("sm", 17), K("sm", 18) + K("sm", 19))
        slots_q = [ring_load(wpiece("h_w_in", (j,), qi * 512, 512), [128, KC, 512], wk) for qi in range(2)]
        slots_f = [None, None]
        for h in range(8):
            if h % 4 == 0:
                slots_f[h // 4] = ring_load(wpiece("h_w_in", (j,), 1024 + (h // 4) * 512, 512), [128, KC, 512], wk)
            sq_ = slots_q[h // 4]
            sf_ = slots_f[h // 4]
            nci = h % 4
            bq = bank(next_bank(1, 6))
            for k in range(KC):
                mm(bq, Buf(sq_.ap[:, k, nci * 128:(nci + 1) * 128], sq_.keys), Buf(hnT[:, k, :], [("hnT", k)]),
                   start=(k == 0), stop=(k == KC - 1))
            qb = Buf(acc[:, h % 2, :], [("acc", h % 2)])
            act(qb, bq, AF.Silu)
            bf_ = bank(next_bank(1, 6))
            for k in range(KC):
                mm(bf_, Buf(sf_.ap[:, k, nci * 128:(nci + 1) * 128], sf_.keys), Buf(hnT[:, k, :], [("hnT", k)]),
                   start=(k == 0), stop=(k == KC - 1))
            fb = 3 * (h % 2)
            f0 = Buf(ft4[:, fb + 0, :], K("ft4", fb + 0))
            f1 = Buf(ft4[:, fb + 1, :], K("ft4", fb + 1))
            f2 = Buf(ft4[:, fb + 2, :], K("ft4", fb + 2))
            act(f0, bf_, AF.Sigmoid)
            act(f0, f0, AF.Identity, scale=omlb[:, j, h:h + 1], bias=lbv[:, j, h:h + 1], extra=K("omlb") + K("lbv"))
            tsc(f0, f0, 1e-30, None, ALU.max)
            act(f1, f0, AF.Ln)
            P.op("dve", lambda e, o_=f2.ap, a_=f1.ap, m_=ft4[:, 6, :]: e.tensor_tensor_scan(
                out=o_, data0=m_, data1=a_, initial=0.0, op0=ALU.mult, op1=ALU.add),
                reads=f1.keys + tuple(K("ft4", 6)), writes=f2.keys)
            B3 = f2.ap.rearrange("p (c n) -> p c n", n=CH)
            cp(Buf(Bref[:, h, :], kB), Buf(B3[:, :, CH // 2 - 1], f2.keys), eng="pool")
            cp(Buf(BLv[:, h, :], kBL), Buf(B3[:, :, CH - 1], f2.keys), eng="pool")
            act(f0, f0, AF.Identity, scale=-1.0, bias=onec[:, 0:1], extra=[("onec",)])
            tt(Buf(B3, f2.keys), Buf(B3, f2.keys),
               Buf(Bref[:, h, :].unsqueeze(2).to_broadcast([128, NCH, CH]), kB), ALU.subtract)
            act(f1, f2, AF.Exp)
            tt(Buf(qkT[:, h, :], [("qk", h)]), qb, f1, ALU.mult)
            act(f1, f2, AF.Exp, scale=-1.0)
            tt(Buf(khT[:, h, :], [("kh", h)]), f0, f1, ALU.mult)
        act(Buf(eref, kER), Buf(Bref, kB), AF.Exp)
        act(Buf(eL, kEL), Buf(BLv, kBL), AF.Exp)
        tt(Buf(eLR, kELR), Buf(BLv, kBL), Buf(Bref, kB), ALU.subtract)
        act(Buf(eLR, kELR), Buf(eLR, kELR), AF.Exp)
        for gi_ in range(2):
            slot = ring_load(wpiece("h_w_in", (j,), 3072 + gi_ * 512, 512), [128, KC, 512], wk)

            def consume(nci, b, gi_=gi_):
                c = gi_ * 4 + nci
                act(Buf(gT[:, c, :], [("gT", c)]), b, AF.Silu)
                tsc(Buf(gT[:, c, :], [("gT", c)]), Buf(gT[:, c, :], [("gT", c)]), hnw[:, j, c:c + 1], None, ALU.mult,
                    extra=K("hnw"))

            proj_fm(slot, 512, consume)
        for vi in range(2):
            slot = ring_load(wpiece("h_w_in", (j,), 2048 + vi * 512, 512), [128, KC, 512], wk)
            for s in range(NS):
                b = bank(next_bank(1, 6))
                for k in range(KC):
                    mm(b, Buf(hnT[:, k, s * 128:(s + 1) * 128], [("hnT", k)]), Buf(slot.ap[:, k, :], slot.keys),
                       start=(k == 0), stop=(k == KC - 1))
                P.op("act", lambda e, o=vt[:, s, vi * 4:(vi + 1) * 4, 0:128],
                     i=b.ap.rearrange("p (h n) -> p h n", h=4): e.activation(out=o, in_=i, func=AF.Copy),
                     reads=b.keys, writes=[("vt", s)])

        def mmt(out, lhsT, rhs, start, stop, tp):
            o, l_, r_ = out.ap, lhsT.ap, rhs.ap
            P.op("pe", lambda e: e.matmul(o, l_, r_, start=start, stop=stop, tile_position=tp, skip_group_check=True),
                 reads=lhsT.keys + rhs.keys, writes=out.keys)

        def body(s):
            gi = ti * NS + s
            pb_ = gi % 2
            t0 = s * 128
            ke = Buf(Sm[:, pb_], [("Sm", pb_)])
            tt(Buf(Sm[:, pb_].rearrange("p h (c n) -> p h c n", c=CPS), ke.keys),
               Buf(khT[:, :, t0:t0 + 128].rearrange("p h (c n) -> p h c n", c=CPS), [("kh", h) for h in range(8)]),
               Buf(eLR[:, :, CPS * s:CPS * s + CPS].unsqueeze(3).to_broadcast([128, 8, CPS, CH]), kELR), ALU.mult)
            tpb = Buf(ps[:, 0, :].bitcast(BF16), [("ps", 0)])
            for h in range(8):
                tr(Buf(ps[:, 0, :].bitcast(BF16)[:, h * 128:(h + 1) * 128], [("ps", 0)]),
                   Buf(Sm[:, pb_, h, :], ke.keys), identb)
            kb = Buf(ktm[:, pb_, :], [("ktm", pb_)])
            cp(kb, tpb, eng="act")
            atk = [("ps", 7)]
            for h in range(8):
                for c in range(CPS):
                    r0 = c * CH
                    mmt(Buf(ps[r0:r0 + CH, 7, h * CH:(h + 1) * CH], atk),
                        Buf(khT[:, h, t0 + r0:t0 + r0 + CH], [("kh", h)]),
                        Buf(qkT[:, h, t0 + r0:t0 + r0 + CH], [("qk", h)]), True, True, (0, r0))
            amk = [("amT", pb_)]
            for c in range(CPS):
                r0 = c * CH
                tt(Buf(amT[r0:r0 + CH, pb_, :, r0:r0 + CH], amk),
                   Buf(ps[r0:r0 + CH, 7, 0:8 * CH].rearrange("p (h n) -> p h n", h=8), atk),
                   Buf(cst[r0:r0 + CH, 5, 0:CH].unsqueeze(1).to_broadcast([CH, 8, CH]), K("cst")), ALU.mult)
            sall = Buf(Sst[:, j], K("Sst", j))
            for c in range(CPS):
                gc = CPS * s + c
                r0 = c * CH
                sbk = [("Sbf", c)]
                db = 5 if c % 2 == 0 else 1
                dck = [("ps", db), ("ps", db + 1)]
                tt(Buf(Sbf[:, c], sbk), sall, Buf(eref[:, :, gc:gc + 1].to_broadcast([128, 8, 128]), kER), ALU.mult)
                for h in range(8):
                    mmt(Buf(ps[:, db + h // 4, (h % 4) * 128:(h % 4 + 1) * 128], dck),
                        Buf(ktm[r0:r0 + CH, pb_, h * 128:(h + 1) * 128], kb.keys),
                        Buf(vt[r0:r0 + CH, s, h, 0:128], [("vt", s)]), True, True, (r0, 0))
                tt(sall, sall, Buf(eL[:, :, gc:gc + 1].to_broadcast([128, 8, 128]), kEL), ALU.mult)
                tt(sall, sall, Buf(ps[:, db:db + 2, :].rearrange("p b (q n) -> p (b q) n", q=4), dck), ALU.add)
            yield
            numk = [("ps", 3), ("ps", 4)]
            for h in range(8):
                o = ps[:, 3 + h // 4, (h % 4) * 128:(h % 4 + 1) * 128]
                mmt(Buf(o, numk), Buf(amT[:, pb_, h, :], amk), Buf(vt[:, s, h, 0:128], [("vt", s)]), True, False, None)
                for c in range(CPS):
                    r0 = c * CH
                    mmt(Buf(ps[r0:r0 + CH, 3 + h // 4, (h % 4) * 128:(h % 4 + 1) * 128], numk),
                        Buf(qkT[:, h, t0 + r0:t0 + r0 + CH], [("qk", h)]), Buf(Sbf[:, c, h, :], [("Sbf", c)]),
                        False, True, (0, r0))
            ov = Buf(ps[:, 3:5, :].rearrange("p b (q n) -> p (b q) n", q=4), numk)
            sqb = Buf(sqn.rearrange("p (h n) -> p h n", h=8), [("ft4", 0), ("ft4", 1)])
            r2 = Buf(small[:, 8, 0:8], K("sm", 8))
            act(sqb, ov, AF.Square)
            red(r2, sqb, ALU.add)
            rsqrt_eps(r2, r2, scale=1.0 / 128.0)
            hb = Buf(hh[:, pb_, :], [("hh", pb_)])
            tt(Buf(hh[:, pb_, :].rearrange("p (h n) -> p h n", h=8), hb.keys), ov,
               Buf(r2.ap.unsqueeze(2).to_broadcast([128, 8, 128]), r2.keys), ALU.mult)
            yield
            for c in range(8):
                tr(Buf(ps[:, 0, :].bitcast(BF16)[:, c * 128:(c + 1) * 128], [("ps", 0)]),
                   Buf(hh[:, pb_, c * 128:(c + 1) * 128], hb.keys), identb)
            tt(Buf(hhnT[:, :, t0:t0 + 128], [("hhnT", c) for c in range(8)]),
               Buf(tpb.ap.rearrange("p (c n) -> p c n", c=8), tpb.keys),
               Buf(gT[:, :, t0:t0 + 128], [("gT", c) for c in range(8)]), ALU.mult)
        run_skewed(body)
        out_proj("h_w_out", j, l)

    def ffn_groups(gname, uname, dname, idx, dff, gate_idx=None):
        out = []
        c0 = 0
        while c0 < dff:
            w = min(512, dff - c0)
            out.append((gname, uname, dname, tuple(idx), c0, w, gate_idx))
            c0 += w
        return out

    ffn_ctr = [0]

    def ffn_run(l, groups):
        pend = None

        def down(sd_, hb_i, ncn):
            for dc in range(KC):
                by = bank(5 + dc % 2)
                for nci in range(ncn):
                    mm(by, Buf(sd_.ap[:, nci, dc * 128:(dc + 1) * 128], sd_.keys),
                       Buf(qkT[:, hb_i * 4 + nci, :], [("qk", hb_i * 4 + nci)]), start=(nci == 0), stop=(nci == ncn - 1))
                resid_add(dc, by, gate2(l, dc), l)

        for (gname, uname, dname, idx, c0, w, gate_idx) in groups:
            ncn = w // 128
            sg_ = ring_load(wpiece(gname, idx, c0, w), [128, KC, w], ("wb", gname) + idx)
            su_ = ring_load(wpiece(uname, idx, c0, w), [128, KC, w], ("wb", uname) + idx)
            hb_i = ffn_ctr[0] % 2
            ffn_ctr[0] += 1
            for nci in range(ncn):
                bg = bank(1 + (nci % 2) * 2)
                bu = bank(2 + (nci % 2) * 2)
                for k in range(KC):
                    mm(bg, Buf(sg_.ap[:, k, nci * 128:(nci + 1) * 128], sg_.keys), Buf(hnT[:, k, :], [("hnT", k)]),
                       start=(k == 0), stop=(k == KC - 1))
                for k in range(KC):
                    mm(bu, Buf(su_.ap[:, k, nci * 128:(nci + 1) * 128], su_.keys), Buf(hnT[:, k, :], [("hnT", k)]),
                       start=(k == 0), stop=(k == KC - 1))
                sgb = Buf(ft4[:, nci % 2, :], K("ft4", nci % 2))
                act(sgb, bg, AF.Silu)
                hk = [("qk", hb_i * 4 + nci)]
                if gate_idx is not None:
                    tt(sgb, sgb, Buf(gbc[:, gate_idx, :], [("tmp", gate_idx)]), ALU.mult, eng="pool")
                tt(Buf(qkT[:, hb_i * 4 + nci, :], hk), sgb, bu, ALU.mult)
            if pend is not None:
                down(*pend)
            a_ = wdst[dname]
            for i in idx:
                a_ = a_[i]
            sd_ = ring_load(a_[c0:c0 + w, :].rearrange("(k p) n -> p k n", p=128), [128, ncn, D], ("wb", dname) + idx)
            pend = (sd_, hb_i, ncn)
        if pend is not None:
            down(*pend)

    def moe(l, ti):
        j = l // 2
        lg = Buf(ps[:, 7, 0:NS * 8].rearrange("p (s n) -> p s n", s=NS), [("ps", 7)])
        for s in range(NS):
            o = Buf(ps[:, 7, s * 8:(s + 1) * 8], [("ps", 7)])
            for k in range(KC):
                mm(o, Buf(tmp[:, k, s * 128:(s + 1) * 128], [("tmp", k)]), Buf(rt[:, j, k, :], K("rt")),
                   start=(k == 0), stop=(k == KC - 1))
        L = Buf(small[:, 0, 0:NS * 8].rearrange("p (s n) -> p s n", s=NS), K("sm", 0))
        cp(L, lg)
        m1 = Buf(small[:, 1, 0:NS], K("sm", 1))
        m2 = Buf(small[:, 2, 0:NS], K("sm", 2))
        eq = Buf(small[:, 3, 0:NS * 8].rearrange("p (s n) -> p s n", s=NS), K("sm", 3))
        pe_ = Buf(small[:, 4, 0:NS * 8].rearrange("p (s n) -> p s n", s=NS), K("sm", 4))
        red(m1, L, ALU.max)
        tt(eq, L, Buf(m1.ap.unsqueeze(2).to_broadcast([128, NS, 8]), m1.keys), ALU.is_equal)
        stt(eq, eq, -1e30, L, ALU.mult, ALU.add)
        red(m2, eq, ALU.max)
        tt(eq, L, Buf(m2.ap.unsqueeze(2).to_broadcast([128, NS, 8]), m2.keys), ALU.is_ge)
        tt(pe_, L, Buf(m1.ap.unsqueeze(2).to_broadcast([128, NS, 8]), m1.keys), ALU.subtract)
        act(pe_, pe_, AF.Exp)
        tt(pe_, pe_, eq, ALU.mult)
        red(m2, pe_, ALU.add)
        P.op("dve", lambda e, o=m2.ap, i=m2.ap: e.reciprocal(out=o, in_=i), reads=m2.keys, writes=m2.keys)
        tt(pe_, pe_, Buf(m2.ap.unsqueeze(2).to_broadcast([128, NS, 8]), m2.keys), ALU.mult)
        for e_ in range(NE):
            b = bank(next_bank(1, 6))
            for s in range(NS):
                mm(Buf(b.ap[:, s * 128:(s + 1) * 128], b.keys),
                   Buf(pe_.ap[:, s, e_:e_ + 1].to_broadcast([128, 128]), pe_.keys), ident32)
            P.op("act", lambda e, o=gbc[:, e_, :], i=b.ap: e.activation(out=o, in_=i, func=AF.Copy),
                 reads=b.keys, writes=[("tmp", e_)])
        groups = []
        for e_ in range(NE):
            groups += ffn_groups("moe_w_gate", "moe_w_up", "moe_w_down", (j, e_), D_FFE, gate_idx=e_)
        ffn_run(l, groups)

    for ti in range(NT):
        r0 = ti * TS
        xin = tmp[:].rearrange("p c t -> p (c t)").rearrange("p (s d) -> p s d", s=NS)
        load(Buf(xin, tmpk), x_d[r0:r0 + TS, :].rearrange("(s p) d -> p s d", p=128), "xin")
        for c in range(KC):
            b = bank(next_bank(1, 6))
            for s in range(NS):
                tr(Buf(b.ap[:, s * 128:(s + 1) * 128], b.keys), Buf(xin[:, s, c * 128:(c + 1) * 128], tmpk), ident32)
            P.op("act", lambda e, o=xT[:, c, :], i=b.ap: e.activation(out=o, in_=i, func=AF.Copy),
                 reads=b.keys, writes=[("xT", c)])
        for l in range(NL):
            if DBG <= 1:
                break
            norm_mod(l, 1, False)
            if DBG <= 2:
                break
            if l % 2 == 0:
                mlstm(l, ti)
            else:
                hgrn(l, ti)
            if half == "mixer" and l == NL - 1:
                break
            if l % 2 == 0:
                norm_mod(l, 2, False)
                ffn_run(l, ffn_groups("ffn_w_gate", "ffn_w_up", "ffn_w_down", (l // 2,), D_FF))
            else:
                norm_mod(l, 2, True)
                moe(l, ti)
        if final:
            rms_stats()
            tt(Buf(tmp[:], tmpk), Buf(xT[:], xk), Buf(rstd[:].unsqueeze(1).to_broadcast([128, KC, TS]), K("rstd")),
               ALU.mult)
            for c in range(KC):
                tsc(Buf(tmp[:, c, :], [("tmp", c)]), Buf(tmp[:, c, :], [("tmp", c)]), fnw[:, c:c + 1], None, ALU.mult,
                    extra=K("fnw"))
            src, srck, stg, stgk = tmp, "tmp", xT, xk
        else:
            src, srck, stg, stgk = xT, "xT", tmp, tmpk
        yout = stg[:].rearrange("p c t -> p (c t)").rearrange("p (s d) -> p s d", s=NS)
        for s in range(NS):
            for half in range(2):
                b = bank(next_bank(1, 6))
                for cc in range(4):
                    c = half * 4 + cc
                    tr(Buf(b.ap[:, cc * 128:(cc + 1) * 128], b.keys), Buf(src[:, c, s * 128:(s + 1) * 128], [(srck, c)]),
                       ident32)
                P.op("act", lambda e, o=yout[:, s, half * 512:(half + 1) * 512], i=b.ap: e.activation(
                    out=o, in_=i, func=AF.Copy), reads=b.keys, writes=stgk)
        P.dma("sp", lambda e, o=out_d[r0:r0 + TS, :].rearrange("(s p) d -> p s d", p=128), i=yout: e.dma_start(
            out=o, in_=i), "out", reads=stgk)

    fch = ["out"]
    if os.environ.get("KDUMP"):
        dbg_d = nc.dram_tensor("dbg", [128, 20 * 64], F32, kind="ExternalOutput").ap()
        P.dma("sp", lambda e: e.dma_start(out=dbg_d, in_=small[:].rearrange("p a n -> p (a n)")), "dbgc",
              reads=[("sm", i) for i in range(20)])
        fch.append("dbgc")
    P.emit(nc, fch)
    st.close()
    return nc


def _fm(v):
    v = np.asarray(v, np.float32)
    lead = v.shape[:-1]
    a = v.reshape(lead + (KC, 128))
    a = np.moveaxis(a, -1, 0)
    return np.ascontiguousarray(a)


def _consts():
    c = np.zeros((128, 6, 128), np.float32)
    i = np.arange(128)
    c[:, 0, :] = np.eye(128, dtype=np.float32)
    tri = (i[:, None] <= i[None, :]).astype(np.float32)
    c[:, 1, :] = tri
    c[:, 2, :] = -tri
    c[:, 3, :] = 1.0
    c[:, 4, :] = 1.0 / 1024.0
    c[:, 5, 0:32] = ((i[:, None] % 32) <= np.arange(32)[None, :]).astype(np.float32)
    return c


def make_in_maps(inputs, NT, cores):
    I = {k: np.asarray(v) for k, v in inputs.items()}
    shared = {
        "ada_w": np.ascontiguousarray(I["ada_w"], np.float32),
        "ada_b": np.ascontiguousarray(np.moveaxis(I["ada_b"].reshape(DEPTH, 48, 128), -1, 0), np.float32),
        "norm1_w": _fm(I["norm1_w"]),
        "norm2_w": _fm(I["norm2_w"]),
        "final_norm_w": _fm(I["final_norm_w"]),
        "m_gate_bias": np.ascontiguousarray(np.broadcast_to(
            np.concatenate([I["m_i_bias"], I["m_f_bias"]], axis=-1)[None], (128, 2, 16)), np.float32),
        "m_conv_w": np.ascontiguousarray(np.transpose(_fm(I["m_conv_w"]), (0, 1, 3, 2)), np.float32),
        "m_conv_b": _fm(I["m_conv_b"]),
        "m_norm_w": _fm(I["m_norm_w"]),
        "h_norm_w": _fm(I["h_norm_w"]),
        "h_lb_logits": _fm(I["h_lb_logits"]),
        "moe_router": np.ascontiguousarray(
            np.transpose(I["moe_router"].reshape(2, KC, 128, NE), (2, 0, 1, 3)), np.float32),
        "consts": _consts(),
    }
    for n in ("m_w_in", "m_w_out", "h_w_in", "h_w_out", "ffn_w_gate", "ffn_w_up", "ffn_w_down",
              "moe_w_gate", "moe_w_up", "moe_w_down"):
        shared[n] = np.ascontiguousarray(I[n], np.float32)
    maps = []
    for b in cores:
        m = dict(shared)
        m["x"] = np.ascontiguousarray(I["x"][b, :NT * TS, :], np.float32)
        m["c"] = _fm(I["c"][b])
        maps.append(m)
    return maps


_NC_CACHE = {}


def run(inputs, NT=SEQ // TS, NL=DEPTH, final=True, cores=tuple(range(8)), trace=False, half="full"):
    key = (NT, NL, final, half)
    if key not in _NC_CACHE:
        _NC_CACHE[key] = build(NT, NL, final, half)
    nc = _NC_CACHE[key]
    maps = make_in_maps(inputs, NT, cores)
    res = run_bass_kernel_spmd(nc, maps, core_ids=list(range(len(cores))), trace=trace)
    out = np.stack([np.asarray(r["out"], np.float32) for r in res.results], axis=0)
    if os.environ.get("KDUMP"):
        np.save(os.environ["KDUMP"], np.asarray(res.results[0]["dbg"]))
    return out, res


def kernel(**inputs):
    out, _ = run(inputs)
    return out.astype(np.float32)
```

```python
import os
import numpy as np
from contextlib import ExitStack
import concourse.bass as bass
import concourse.mybir as mybir
from concourse.bass_utils import run_bass_kernel_spmd

F32 = mybir.dt.float32
BF16 = mybir.dt.bfloat16
AF = mybir.ActivationFunctionType
ALU = mybir.AluOpType
AX = mybir.AxisListType

D = 1024
KC = 8
SEQ = 4096
TS = 512
NS = TS // 128
DEPTH = 4
M_PROJ = 3088
HG_PROJ = 4096
D_FF = 2816
NE = 8
D_FFE = 1408
EPS = 1e-6
DBG = float(os.environ.get("KDBG", "99"))
NSLOT = 6
NCAST = 6


class Buf:
    __slots__ = ("ap", "keys")

    def __init__(self, ap, keys):
        self.ap = ap
        self.keys = tuple(keys)


class Prog:
    ENG = ("pe", "act", "dve", "pool", "sp")

    def __init__(self):
        self.ops = {e: [] for e in self.ENG}
        self.tick = {e: 0 for e in self.ENG}
        self.chan = {}
        self.lastw = {}
        self.readers = {}
        self.seen = {e: {} for e in self.ENG}

    def _deps(self, eng, reads, writes):
        deps = {}

        def add(ev):
            if ev is None:
                return
            s, v = ev
            if deps.get(s, 0) < v:
                deps[s] = v

        for k in reads:
            add(self.lastw.get(k))
            if k[0] == "ps":
                for s, v in self.readers.get(k, {}).items():
                    if s != eng:
                        add((s, v))
        for k in writes:
            add(self.lastw.get(k))
            for s, v in self.readers.get(k, {}).items():
                add((s, v))
        waits = []
        seen = self.seen[eng]
        for s, v in deps.items():
            if s == "pe" and eng == "pe":
                continue
            if seen.get(s, 0) >= v:
                continue
            seen[s] = v
            waits.append((s, v))
        return waits

    def _commit(self, ev, reads, writes):
        s, v = ev
        for k in reads:
            self.readers.setdefault(k, {})[s] = v
        for k in writes:
            self.lastw[k] = ev
            self.readers[k] = {}

    def op(self, eng, fn, reads=(), writes=()):
        waits = self._deps(eng, reads, writes)
        self.tick[eng] += 1
        ev = (eng, self.tick[eng])
        self.ops[eng].append((fn, waits, ev, False))
        self._commit(ev, reads, writes)

    def dma(self, eng, fn, chan, reads=(), writes=()):
        writes = tuple(writes) + (("chan", chan),)
        waits = self._deps(eng, reads, writes)
        self.chan[chan] = self.chan.get(chan, 0) + 16
        ev = ("d:" + chan, self.chan[chan])
        self.ops[eng].append((fn, waits, ev, True))
        self._commit(ev, reads, writes)

    def emit(self, nc, final_chans):
        with ExitStack() as st:
            sems = {}
            for n in list(self.ENG) + ["d:" + c for c in self.chan]:
                sems[n] = st.enter_context(nc.semaphore("s_" + n.replace(":", "_")))
            block = st.enter_context(nc.Block())

            def run(name, e):
                for fn, waits, ev, isdma in self.ops[name]:
                    for s, v in waits:
                        e.wait_ge(sems[s], v)
                    ins = fn(e)
                    ins.then_inc(sems[ev[0]], 16 if isdma else 1)
                if name == "sp":
                    for c in final_chans:
                        e.wait_ge(sems["d:" + c], self.chan[c])

            @block.tensor
            def _(e):
                run("pe", e)

            @block.scalar
            def _(e):
                run("act", e)

            @block.vector
            def _(e):
                run("dve", e)

            @block.gpsimd
            def _(e):
                run("pool", e)

            @block.sync
            def _(e):
                run("sp", e)


def build(NT=8, NL=DEPTH, final=True, half="full"):
    nc = bass.Bass("TRN2", target_bir_lowering=False)
    P = Prog()
    st = ExitStack()

    def din(name, shape, dt=F32):
        return nc.dram_tensor(name, list(shape), dt, kind="ExternalInput").ap()

    x_d = din("x", [NT * TS, D])
    out_d = nc.dram_tensor("out", [NT * TS, D], F32, kind="ExternalOutput").ap()
    c_d = din("c", [128, KC])
    ada_w_d = din("ada_w", [DEPTH, D, 6 * D])
    ada_b_d = din("ada_b", [128, DEPTH, 48])
    n1_d = din("norm1_w", [128, DEPTH, KC])
    n2_d = din("norm2_w", [128, DEPTH, KC])
    fn_d = din("final_norm_w", [128, KC])
    gb_d = din("m_gate_bias", [128, 2, 16])
    cw_d = din("m_conv_w", [128, 2, KC, 4])
    cb_d = din("m_conv_b", [128, 2, KC])
    mnw_d = din("m_norm_w", [128, 2, KC])
    hnw_d = din("h_norm_w", [128, 2, KC])
    lb_d = din("h_lb_logits", [128, 2, KC])
    rt_d = din("moe_router", [128, 2, KC, NE])
    cst_d = din("consts", [128, 6, 128])
    wshapes = {
        "m_w_in": [2, D, M_PROJ], "m_w_out": [2, D, D], "h_w_in": [2, D, HG_PROJ], "h_w_out": [2, D, D],
        "ffn_w_gate": [2, D, D_FF], "ffn_w_up": [2, D, D_FF], "ffn_w_down": [2, D_FF, D],
        "moe_w_gate": [2, NE, D, D_FFE], "moe_w_up": [2, NE, D, D_FFE], "moe_w_down": [2, NE, D_FFE, D],
    }
    wsrc = {}
    wdst = {}
    for n, shp in wshapes.items():
        wsrc[n] = din(n, shp)
        wdst[n] = nc.dram_tensor(n + "_b", list(shp), BF16, kind="Internal").ap()

    def sb(name, shape, dt=F32):
        return st.enter_context(nc.sbuf_tensor(name, list(shape), dt))

    ps = st.enter_context(nc.psum_tensor("ps", [128, 8, 512], F32))

    def bank(i):
        return Buf(ps[:, i, :], [("ps", i)])

    xT = sb("xT", [128, KC, TS])
    tmp = sb("tmp", [128, KC, TS])
    hnT = sb("hnT", [128, KC, TS], BF16)
    hhnT = sb("hhnT", [128, KC, TS], BF16)
    sq = hhnT
    rstd = sb("rstd", [128, TS])
    ring = sb("ring", [128, NSLOT, 4096], BF16)
    qkT = sb("qkT", [128, KC, TS], BF16)
    khT = sb("khT", [128, KC, TS], BF16)
    gT = sb("gT", [128, KC, TS], BF16)
    vt = sb("vt", [128, NS, 8, 129], BF16)
    cv = sb("cv", [128, 2, TS + 3])
    acc = sb("acc", [128, 2, TS])
    ktm = sb("ktm", [128, 2, D], BF16)
    Sm = sb("Sm", [128, 2, 8, 128], BF16)
    hh = sb("hh", [128, 2, D], BF16)
    ft4 = sb("ft4", [128, 7, TS])
    sqn = ft4[:, 0:2, :].rearrange("p a n -> p (a n)")
    small = sb("small", [128, 20, 64])
    Cst = sb("Cst", [128, 2, 4, 129])
    Cbf = sb("Cbf", [128, 2, 2, 4, 129], BF16)
    Sst = sb("Sst", [128, 2, 8, 128])
    Sbf = sb("Sbf", [128, 4, 8, 128], BF16)
    amT = sb("amT", [128, 2, 8, 128], BF16)
    hist = sb("hist", [128, 2, KC, 3])
    cst = sb("cst", [128, 6, 128])
    cstb = sb("cstb", [128, 2, 128], BF16)
    cond = sb("cond", [128, KC])
    epsc = sb("epsc", [128, 1])
    l8c = sb("l8c", [128, 1])
    onec = sb("onec", [128, 1])
    modv = sb("modv", [128, DEPTH, 48])
    adabias = sb("adabias", [128, DEPTH, 48])
    n1 = sb("n1", [128, DEPTH, KC])
    n2 = sb("n2", [128, DEPTH, KC])
    fnw = sb("fnw", [128, KC])
    A1 = sb("A1", [128, DEPTH, KC])
    A2 = sb("A2", [128, DEPTH, KC])
    gbias = sb("gbias", [128, 2, 16])
    cw = sb("cw", [128, 2, KC, 4])
    cb = sb("cb", [128, 2, KC])
    mnw = sb("mnw", [128, 2, KC])
    hnw = sb("hnw", [128, 2, KC])
    lbl = sb("lbl", [128, 2, KC])
    lbv = sb("lbv", [128, 2, KC])
    omlb = sb("omlb", [128, 2, KC])
    rt = sb("rt", [128, 2, KC, NE])
    gbc = tmp

    def mm(out, lhsT, rhs, start=True, stop=True):
        o, l, r = out.ap, lhsT.ap, rhs.ap
        P.op("pe", lambda e: e.matmul(o, l, r, start=start, stop=stop),
             reads=lhsT.keys + rhs.keys, writes=out.keys)

    def tr(out, in_, ident):
        o, i, d = out.ap, in_.ap, ident.ap
        P.op("pe", lambda e: e.transpose(o, i, d), reads=in_.keys + ident.keys, writes=out.keys)

    def act(out, in_, func, scale=1.0, bias=0.0, extra=()):
        o, i = out.ap, in_.ap
        P.op("act", lambda e: e.activation(out=o, in_=i, func=func, scale=scale, bias=bias),
             reads=in_.keys + tuple(extra), writes=out.keys)

    def tt(out, a, b, op, eng="dve"):
        o, x0, x1 = out.ap, a.ap, b.ap
        P.op(eng, lambda e: e.tensor_tensor(out=o, in0=x0, in1=x1, op=op),
             reads=a.keys + b.keys, writes=out.keys)

    def tsc(out, a, s1, s2, op0, op1=None, extra=(), eng="dve"):
        o, x0 = out.ap, a.ap
        if op1 is None:
            P.op(eng, lambda e: e.tensor_scalar(out=o, in0=x0, scalar1=s1, scalar2=None, op0=op0),
                 reads=a.keys + tuple(extra), writes=out.keys)
        else:
            P.op(eng, lambda e: e.tensor_scalar(out=o, in0=x0, scalar1=s1, scalar2=s2, op0=op0, op1=op1),
                 reads=a.keys + tuple(extra), writes=out.keys)

    def stt(out, a, s, b, op0, op1, extra=(), eng="dve"):
        o, x0, x1 = out.ap, a.ap, b.ap
        P.op(eng, lambda e: e.scalar_tensor_tensor(out=o, in0=x0, scalar=s, in1=x1, op0=op0, op1=op1),
             reads=a.keys + b.keys + tuple(extra), writes=out.keys)

    def rsqrt_eps(out, in_, scale=1.0):
        act(out, in_, AF.Sqrt, scale=scale, bias=epsc[:out.ap.shape[0], 0:1], extra=[("epsc",)])
        o = out.ap
        P.op("dve", lambda e: e.reciprocal(out=o, in_=o), reads=out.keys, writes=out.keys)

    def cp(out, in_, eng="dve"):
        o, i = out.ap, in_.ap
        if eng == "act":
            P.op("act", lambda e: e.activation(out=o, in_=i, func=AF.Copy), reads=in_.keys, writes=out.keys)
        else:
            P.op(eng, lambda e: e.tensor_copy(out=o, in_=i), reads=in_.keys, writes=out.keys)

    def red(out, in_, op, eng="dve"):
        o, i = out.ap, in_.ap
        P.op(eng, lambda e: e.tensor_reduce(out=o, in_=i, axis=AX.X, op=op), reads=in_.keys, writes=out.keys)

    def memset(out, val, eng="dve"):
        o = out.ap
        P.op(eng, lambda e: e.memset(o, val), writes=out.keys)

    def load(dst, src_ap, chan, eng="sp", reads=()):
        o = dst.ap
        P.dma(eng, lambda e: e.dma_start(out=o, in_=src_ap), chan, reads=reads, writes=dst.keys)

    ring_ctr = [0]

    def ring_load(src_ap, shape, wkey):
        i = ring_ctr[0] % NSLOT
        ring_ctr[0] += 1
        n = int(np.prod(shape[1:]))
        if len(shape) == 3:
            view = ring[:, i, 0:n].rearrange("p (k n) -> p k n", k=shape[1])
        else:
            view = ring[:, i, 0:n]
        b = Buf(view, [("ring", i)])
        load(b, src_ap, "ring%d" % i, reads=[wkey])
        return b

    def wpiece(name, idx, c0, w):
        a = wdst[name]
        for i in idx:
            a = a[i]
        return a[:, c0:c0 + w].rearrange("(k p) n -> p k n", p=128)

    cast_ctr = [0]

    def cast(name, idx):
        s, d_ = wsrc[name], wdst[name]
        for i in idx:
            s, d_ = s[i], d_[i]
        sf = s.rearrange("r c -> (r c)").rearrange("(a b) -> a b", b=2048)
        df = d_.rearrange("r c -> (r c)").rearrange("(a b) -> a b", b=2048)
        ch = "cast%d" % (cast_ctr[0] % NCAST)
        cast_ctr[0] += 1
        P.dma("pool", lambda e: e.dma_start(out=df, in_=sf), ch, writes=[("wb", name) + tuple(idx)])

    for j in range(2):
        if 2 * j < NL:
            for n in ("m_w_in", "m_w_out", "ffn_w_gate", "ffn_w_up", "ffn_w_down"):
                cast(n, (j,))
        if 2 * j + 1 < NL:
            for n in ("h_w_in", "h_w_out"):
                cast(n, (j,))
            for e_ in range(NE):
                for n in ("moe_w_gate", "moe_w_up", "moe_w_down"):
                    cast(n, (j, e_))

    def K(name, *idx):
        return [(name,) + tuple(idx)] if idx else [(name,)]

    def cload(tile, src, name):
        load(Buf(tile[:], K(name)), src, "const")

    cload(cst, cst_d, "cst")
    cload(cond, c_d, "cond")
    cload(adabias, ada_b_d, "adabias")
    cload(n1, n1_d, "n1")
    cload(n2, n2_d, "n2")
    cload(fnw, fn_d, "fnw")
    cload(gbias, gb_d, "gbias")
    cload(cw, cw_d, "cw")
    cload(cb, cb_d, "cb")
    cload(mnw, mnw_d, "mnw")
    cload(hnw, hnw_d, "hnw")
    cload(lbl, lb_d, "lbl")
    cload(rt, rt_d, "rt")
    ident32 = Buf(cst[:, 0, :], K("cst"))
    mask32 = Buf(cst[:, 1, :], K("cst"))
    tripos = Buf(cst[:, 1, :], K("cst"))
    onespos = Buf(cst[:, 3, :], K("cst"))
    mask64 = Buf(cst[:, 5, :], K("cst"))
    cp(Buf(cstb[:, 0, :], K("cstb")), Buf(cst[:, 0, :], K("cst")))
    cp(Buf(cstb[:, 1, :], K("cstb")), Buf(cst[:, 4, :], K("cst")))
    identb = Buf(cstb[:, 0, :], K("cstb"))
    onesdiv = Buf(cstb[:, 1, :], K("cstb"))

    memset(Buf(epsc[:], [("epsc",)]), EPS)
    memset(Buf(l8c[:], [("l8c",)]), float(np.log(0.125)))
    memset(Buf(onec[:], [("onec",)]), 1.0)
    act(Buf(cond[:], K("cond")), Buf(cond[:], K("cond")), AF.Silu)
    memset(Buf(lbv[:, 0, :], K("lbv")), 0.0)
    tt(Buf(lbv[:, 1, :], K("lbv")), Buf(lbl[:, 1, :], K("lbl")), Buf(lbl[:, 0, :], K("lbl")), ALU.subtract)
    act(Buf(lbv[:, 1, :], K("lbv")), Buf(lbv[:, 1, :], K("lbv")), AF.Sigmoid)
    tsc(Buf(omlb[:], K("omlb")), Buf(lbv[:], K("lbv")), -1.0, 1.0, ALU.mult, ALU.add)
    memset(Buf(Cst[:], [("Cst", 0), ("Cst", 1)]), 0.0)
    memset(Buf(Cbf[:], [("Cbf", a_, b_) for a_ in range(2) for b_ in range(2)]), 0.0)
    memset(Buf(Sst[:], [("Sst", 0), ("Sst", 1)]), 0.0)
    memset(Buf(hist[:], K("hist")), 0.0)
    memset(Buf(small[:], [("sm", i_) for i_ in range(20)]), 0.0)
    memset(Buf(amT[:], [("amT", 0), ("amT", 1)]), 0.0)
    memset(Buf(vt[:], [("vt", s_) for s_ in range(NS)]), 1.0)
    memset(Buf(ft4[:, 6, :], K("ft4", 6)), 1.0)
    memset(Buf(ft4[:, 6, 0:TS:32], K("ft4", 6)), 0.0)

    for l in range(NL):
        for g in range(12):
            bslot = g % 3
            adab_v = ring[:, 2 * bslot:2 * bslot + 2, :].rearrange("p a n -> p (a n)").bitcast(F32).rearrange(
                "p (k n) -> p k n", k=KC)
            adk = [("ring", 2 * bslot), ("ring", 2 * bslot + 1)]
            dst = Buf(adab_v, adk)
            load(dst, ada_w_d[l, :, g * 512:(g + 1) * 512].rearrange("(k p) n -> p k n", p=128), "ada%d" % bslot)
            for nci in range(4):
                col = g * 4 + nci
                o = Buf(ps[:, 7, col:col + 1], [("ps", 7)])
                for k in range(KC):
                    mm(o, Buf(adab_v[:, k, nci * 128:(nci + 1) * 128], adk),
                       Buf(cond[:, k:k + 1], K("cond")), start=(k == 0), stop=(k == KC - 1))
        tt(Buf(modv[:, l, :], K("modv", l)), Buf(ps[:, 7, 0:48], [("ps", 7)]),
           Buf(adabias[:, l, :], K("adabias")), ALU.add)
        stt(Buf(A1[:, l, :], K("A1", l)), Buf(modv[:, l, 8:16], K("modv", l)), 1.0, Buf(n1[:, l, :], K("n1")),
            ALU.add, ALU.mult)
        stt(Buf(A2[:, l, :], K("A2", l)), Buf(modv[:, l, 32:40], K("modv", l)), 1.0, Buf(n2[:, l, :], K("n2")),
            ALU.add, ALU.mult)

    def shift1(l, c):
        return modv[:, l, 0 + c:1 + c]

    def gate1(l, c):
        return modv[:, l, 16 + c:17 + c]

    def shift2(l, c):
        return modv[:, l, 24 + c:25 + c]

    def gate2(l, c):
        return modv[:, l, 40 + c:41 + c]

    xk = [("xT", c) for c in range(KC)]
    hnk = [("hnT", c) for c in range(KC)]
    tmpk = [("tmp", c) for c in range(KC)]

    def rms_stats():
        for k in range(KC):
            if k % 2 == 0:
                act(Buf(sq[:, k, :], [("hhnT", k)]), Buf(xT[:, k, :], [("xT", k)]), AF.Square)
            else:
                tt(Buf(sq[:, k, :], [("hhnT", k)]), Buf(xT[:, k, :], [("xT", k)]), Buf(xT[:, k, :], [("xT", k)]), ALU.mult)
            mm(bank(0), onesdiv, Buf(sq[:, k, :], [("hhnT", k)]), start=(k == 0), stop=(k == KC - 1))
        rsqrt_eps(Buf(rstd[:], K("rstd")), bank(0))

    def norm_mod(l, which, keep32):
        rms_stats()
        A = A1 if which == 1 else A2
        for c in range(KC):
            tt(Buf(tmp[:, c, :], [("tmp", c)]), Buf(xT[:, c, :], [("xT", c)]), Buf(rstd[:], K("rstd")), ALU.mult)
            sh = shift1(l, c) if which == 1 else shift2(l, c)
            extra = K("A1" if which == 1 else "A2", l) + K("modv", l)
            if keep32:
                act(Buf(tmp[:, c, :], [("tmp", c)]), Buf(tmp[:, c, :], [("tmp", c)]), AF.Identity,
                    scale=A[:, l, c:c + 1], bias=sh, extra=extra)
                cp(Buf(hnT[:, c, :], [("hnT", c)]), Buf(tmp[:, c, :], [("tmp", c)]), eng="pool")
            else:
                act(Buf(hnT[:, c, :], [("hnT", c)]), Buf(tmp[:, c, :], [("tmp", c)]), AF.Identity,
                    scale=A[:, l, c:c + 1], bias=sh, extra=extra)

    def resid_add(dc, psb, gate_ap, l):
        stt(Buf(xT[:, dc, :], [("xT", dc)]), psb, gate_ap, Buf(xT[:, dc, :], [("xT", dc)]), ALU.mult, ALU.add,
            extra=K("modv", l))

    pbc = [0]

    def next_bank(lo, n):
        i = lo + pbc[0] % n
        pbc[0] += 1
        return i

    def proj_fm(slot, ncols, consume):
        for nci in range(ncols // 128):
            b = bank(next_bank(1, 6))
            for k in range(KC):
                mm(b, Buf(slot.ap[:, k, nci * 128:(nci + 1) * 128], slot.keys), Buf(hnT[:, k, :], [("hnT", k)]),
                   start=(k == 0), stop=(k == KC - 1))
            consume(nci, b)

    def out_proj(name, j, l):
        for g in range(2):
            slot = ring_load(wpiece(name, (j,), g * 512, 512), [128, KC, 512], ("wb", name, j))
            for nci in range(4):
                dc = g * 4 + nci
                b = bank(next_bank(1, 6))
                for k in range(KC):
                    mm(b, Buf(slot.ap[:, k, nci * 128:(nci + 1) * 128], slot.keys),
                       Buf(hhnT[:, k, :], [("hhnT", k)]), start=(k == 0), stop=(k == KC - 1))
                resid_add(dc, b, gate1(l, dc), l)

    def run_skewed(body):
        gens = [body(s_) for s_ in range(NS)]
        next(gens[0])
        for s_ in range(NS):
            next(gens[s_])
            if s_ + 1 < NS:
                next(gens[s_ + 1])
            for _ in gens[s_]:
                pass

    def mlstm(l, ti):
        j = l // 2
        wk = ("wb", "m_w_in", j)
        slot = ring_load(wpiece("m_w_in", (j,), 3072, 16), [128, KC, 16], wk)
        gps = Buf(ps[:, 7, 0:NS * 16].rearrange("p (s n) -> p s n", s=NS), [("ps", 7)])
        for s in range(NS):
            o = Buf(ps[:, 7, s * 16:(s + 1) * 16], [("ps", 7)])
            for k in range(KC):
                mm(o, Buf(hnT[:, k, s * 128:(s + 1) * 128], [("hnT", k)]), Buf(slot.ap[:, k, :], slot.keys),
                   start=(k == 0), stop=(k == KC - 1))
        G = small[:, 0, 0:NS * 16].rearrange("p (s n) -> p s n", s=NS)
        tt(Buf(G, K("sm", 0)), gps, Buf(gbias[:, j, :].unsqueeze(1).to_broadcast([128, NS, 16]), K("gbias")),
           ALU.add)
        if DBG <= 2.2:
            return
        th = small[:, 1, 0:NS * 8].rearrange("p (s n) -> p s n", s=NS)
        act(Buf(th, K("sm", 1)), Buf(G[:, :, 0:8], K("sm", 0)), AF.Tanh, scale=1.0 / 15.0)
        spv = small[:, 2, 0:NS * 8].rearrange("p (s n) -> p s n", s=NS)
        act(Buf(spv, K("sm", 2)), Buf(G[:, :, 8:16], K("sm", 0)), AF.Exp, scale=-1.0)
        act(Buf(spv, K("sm", 2)), Buf(spv, K("sm", 2)), AF.Ln, bias=1.0)
        if DBG <= 2.4:
            return
        bps = ps[:, 7, 64:64 + NS * 16].rearrange("p (s n) -> p s n", s=NS)
        for s in range(NS):
            mm(Buf(ps[:, 7, 64 + s * 16:64 + s * 16 + 8], [("ps", 7)]), tripos, Buf(spv[:, s, :], K("sm", 2)))
            mm(Buf(ps[:, 7, 64 + s * 16 + 8:64 + s * 16 + 16], [("ps", 7)]), onespos, Buf(spv[:, s, :], K("sm", 2)))
        if DBG <= 2.6:
            return
        bps_b = Buf(bps, [("ps", 7)])
        eb = small[:, 3, 0:NS * 16].rearrange("p (s n) -> p s n", s=NS)
        act(Buf(eb, K("sm", 3)), bps_b, AF.Exp, scale=-1.0)
        if DBG <= 2.7:
            return
        wv = small[:, 4, 0:NS * 8].rearrange("p (s n) -> p s n", s=NS)
        bsb = small[:, 11, 0:NS * 16].rearrange("p (s n) -> p s n", s=NS)
        cp(Buf(bsb, K("sm", 11)), bps_b)
        stt(Buf(wv, K("sm", 4)), Buf(th, K("sm", 1)), 15.0, Buf(bsb[:, :, 0:8], K("sm", 11)), ALU.mult, ALU.add)
        act(Buf(wv, K("sm", 4)), Buf(wv, K("sm", 4)), AF.Exp, bias=l8c[:, 0:1], extra=[("l8c",)])
        if DBG <= 2.8:
            return
        EL = small[:, 5, 0:NS * 4].rearrange("p (s n) -> p s n", s=NS)
        cp(Buf(EL[0:64], K("sm", 5)), Buf(eb[0:64, :, 8:16:2], K("sm", 3)))
        cp(Buf(EL[64:128], K("sm", 5)), Buf(eb[64:128, :, 9:16:2], K("sm", 3)))

        if DBG <= 3:
            return
        pend_silu = []
        for qi in range(2):
            slot = ring_load(wpiece("m_w_in", (j,), qi * 512, 512), [128, KC, 512], wk)

            def consume(nci, b, qi=qi):
                c = qi * 4 + nci
                cb_ = c % 2
                cvb = Buf(cv[:, cb_, :], [("cv", cb_)])
                act(Buf(cv[:, cb_, 3:3 + TS], [("cv", cb_)]), b, AF.Copy)
                while pend_silu:
                    pend_silu.pop(0)()
                cp(Buf(cv[:, cb_, 0:3], [("cv", cb_)]), Buf(hist[:, j, c, :], K("hist")), eng="pool")
                ab = Buf(acc[:, cb_, :], [("acc", cb_)])
                tsc(ab, Buf(cv[:, cb_, 0:TS], cvb.keys), cw[:, j, c, 0:1], cb[:, j, c:c + 1], ALU.mult, ALU.add,
                    extra=K("cw") + K("cb"))
                for tap in range(1, 4):
                    stt(ab, Buf(cv[:, cb_, tap:tap + TS], cvb.keys), cw[:, j, c, tap:tap + 1], ab, ALU.mult, ALU.add,
                        extra=K("cw"))
                cp(Buf(hist[:, j, c, :], K("hist")), Buf(cv[:, cb_, TS:TS + 3], cvb.keys), eng="pool")
                pend_silu.append(lambda c=c, ab=ab: act(Buf(qkT[:, c, :], [("qk", c)]), ab, AF.Silu))

            proj_fm(slot, 512, consume)
        while pend_silu:
            pend_silu.pop(0)()
        if DBG <= 4:
            return
        for oi in range(2):
            slot = ring_load(wpiece("m_w_in", (j,), 2048 + oi * 512, 512), [128, KC, 512], wk)

            def consume(nci, b, oi=oi):
                c = oi * 4 + nci
                act(Buf(gT[:, c, :], [("gT", c)]), b, AF.Sigmoid)
                tsc(Buf(gT[:, c, :], [("gT", c)]), Buf(gT[:, c, :], [("gT", c)]), mnw[:, j, c:c + 1], None, ALU.mult,
                    extra=K("mnw"))

            proj_fm(slot, 512, consume)
        for vi in range(2):
            slot = ring_load(wpiece("m_w_in", (j,), 1024 + vi * 512, 512), [128, KC, 512], wk)
            for s in range(NS):
                b = bank(next_bank(1, 6))
                for k in range(KC):
                    mm(b, Buf(hnT[:, k, s * 128:(s + 1) * 128], [("hnT", k)]), Buf(slot.ap[:, k, :], slot.keys),
                       start=(k == 0), stop=(k == KC - 1))
                tt(Buf(vt[:, s, vi * 4:(vi + 1) * 4, 0:128], [("vt", s)]),
                   Buf(b.ap.rearrange("p (h n) -> p h n", h=4), b.keys),
                   Buf(wv[:, s, vi * 4:(vi + 1) * 4].unsqueeze(2).to_broadcast([128, 4, 128]), K("sm", 4)), ALU.mult)
        for s in range(NS):
            cp(Buf(vt[:, s, :, 128], [("vt", s)]), Buf(wv[:, s, :], K("sm", 4)))

        if DBG <= 5:
            return
        def body(s):
            gi = ti * NS + s
            pb_ = gi % 2
            t0 = s * 128
            ktp = Buf(ps[:, 0, :].bitcast(BF16)[:, 0:512], [("ps", 0)])
            for c in range(4):
                tr(Buf(ps[:, 0, :].bitcast(BF16)[:, c * 128:(c + 1) * 128], [("ps", 0)]),
                   Buf(qkT[:, 4 + c, t0:t0 + 128], [("qk", 4 + c)]), identb)
            kb = Buf(ktm[:, pb_, 0:512], [("ktm", pb_)])
            cp(kb, ktp, eng="act")
            stb = Buf(ps[:, 1:3, :].rearrange("p b n -> p (b n)"), [("ps", 1), ("ps", 2)])
            for h in range(8):
                c, po = h // 2, (h % 2) * 64
                mm(Buf(ps[:, 1 + h % 2, (h // 2) * 128:(h // 2 + 1) * 128], stb.keys),
                   Buf(qkT[po:po + 64, 4 + c, t0:t0 + 128], [("qk", 4 + c)]),
                   Buf(qkT[po:po + 64, c, t0:t0 + 128], [("qk", c)]))
            smb = Buf(Sm[:, pb_], [("Sm", pb_)])
            tt(Buf(Sm[:, pb_].rearrange("p (pr par) n -> p par pr n", par=2), smb.keys),
               Buf(ps[:, 1:3, :].rearrange("p b (q n) -> p b q n", q=4), stb.keys),
               Buf(mask32.ap.unsqueeze(1).unsqueeze(1).to_broadcast([128, 2, 4, 128]), mask32.keys), ALU.mult)
            dck = [("ps", 6), ("ps", 7)]
            for h in range(8):
                pr, po = h // 2, (h % 2) * 64
                mm(Buf(ps[po:po + 64, 6 + pr // 2, (pr % 2) * 256:(pr % 2) * 256 + 129], dck),
                   Buf(ktm[:, pb_, h * 64:(h + 1) * 64], kb.keys), Buf(vt[:, s, h, :], [("vt", s)]))
            cprev = gi % 2
            dcv = Buf(ps[:, 6:8, :].rearrange("p b (q n) -> p (b q) n", q=2)[:, :, 0:129], dck)
            cs = Buf(Cst[:, j], K("Cst", j))
            tt(cs, cs, dcv, ALU.add)
            tt(cs, cs, Buf(EL[:, s, :].unsqueeze(2).to_broadcast([128, 4, 129]), K("sm", 5)), ALU.mult)
            cp(Buf(Cbf[:, j, 1 - cprev], [("Cbf", j, 1 - cprev)]), cs, eng="pool")
            yield
            numk = [("ps", 3), ("ps", 4)]
            denk = [("ps", 5)]
            cprev = gi % 2
            for h in range(8):
                c, po, pr = h // 2, (h % 2) * 64, h // 2
                o = Buf(ps[:, 3 + h // 4, (h % 4) * 128:(h % 4 + 1) * 128], numk)
                mm(o, Buf(Sm[:, pb_, h, :], smb.keys), Buf(vt[:, s, h, 0:128], [("vt", s)]), start=True, stop=False)
                mm(o, Buf(qkT[po:po + 64, c, t0:t0 + 128], [("qk", c)]),
                   Buf(Cbf[po:po + 64, j, cprev, pr, 0:128], [("Cbf", j, cprev)]), start=False, stop=True)
            for h in range(8):
                c, po, pr = h // 2, (h % 2) * 64, h // 2
                o = Buf(ps[:, 5, h:h + 1], denk)
                mm(o, Buf(Sm[:, pb_, h, :], smb.keys), Buf(vt[:, s, h, 128:129], [("vt", s)]), start=True, stop=False)
                mm(o, Buf(qkT[po:po + 64, c, t0:t0 + 128], [("qk", c)]),
                   Buf(Cbf[po:po + 64, j, cprev, pr, 128:129], [("Cbf", j, cprev)]), start=False, stop=True)
            numv = Buf(ps[:, 3:5, :].rearrange("p b (q n) -> p (b q) n", q=4), numk)
            den = Buf(ps[:, 5, 0:8], denk)
            ebs = Buf(eb[:, s, 0:8], K("sm", 3))
            r0 = Buf(small[:, 6, 0:8], K("sm", 6))
            r1 = Buf(small[:, 7, 0:8], K("sm", 7))
            r2 = Buf(small[:, 8, 0:8], K("sm", 8))
            r3 = Buf(small[:, 9, 0:8], K("sm", 9))
            act(r0, den, AF.Abs)
            tt(r0, r0, ebs, ALU.mult)
            tsc(r0, r0, 1.0, None, ALU.max)
            P.op("dve", lambda e, o=r1.ap, i=r0.ap: e.reciprocal(out=o, in_=i), reads=r0.keys, writes=r1.keys)
            tt(r1, r1, ebs, ALU.mult)
            sqb = Buf(sqn.rearrange("p (h n) -> p h n", h=8), [("ft4", 0), ("ft4", 1)])
            act(sqb, numv, AF.Square)
            red(r2, sqb, ALU.add)
            tt(r3, r1, r1, ALU.mult)
            tt(r2, r2, r3, ALU.mult)
            rsqrt_eps(r2, r2, scale=1.0 / 128.0)
            tt(r2, r2, r1, ALU.mult)
            hb = Buf(hh[:, pb_, :], [("hh", pb_)])
            tt(Buf(hh[:, pb_, :].rearrange("p (h n) -> p h n", h=8), hb.keys), numv,
               Buf(r2.ap.unsqueeze(2).to_broadcast([128, 8, 128]), r2.keys), ALU.mult)
            yield
            tpb = Buf(ps[:, 0, :].bitcast(BF16), [("ps", 0)])
            for c in range(8):
                tr(Buf(ps[:, 0, :].bitcast(BF16)[:, c * 128:(c + 1) * 128], [("ps", 0)]),
                   Buf(hh[:, pb_, c * 128:(c + 1) * 128], hb.keys), identb)
            tt(Buf(hhnT[:, :, t0:t0 + 128], [("hhnT", c) for c in range(8)]),
               Buf(tpb.ap.rearrange("p (c n) -> p c n", c=8), tpb.keys),
               Buf(gT[:, :, t0:t0 + 128], [("gT", c) for c in range(8)]), ALU.mult)
        run_skewed(body)
        out_proj("m_w_out", j, l)

    def hgrn(l, ti):
        j = l // 2
        wk = ("wb", "h_w_in", j)
        CH = 32
        NCH = TS // CH
        CPS = 128 // CH

        def sm2(i):
            return small[:, i:i + 2, :].rearrange("p a n -> p (a n)").rearrange("p (h c) -> p h c", h=8)

        Bref, BLv, eref, eL, eLR = sm2(10), sm2(12), sm2(14), sm2(16), sm2(18)
        kB, kBL, kER, kEL, kELR = (K("sm", 10) + K("sm", 11), K("sm", 12) + K("sm", 13), K("sm", 14) + K("sm", 15),
                                   K("sm", 16) + K("sm", 17), K("sm", 18) + K("sm", 19))
        slots_q = [ring_load(wpiece("h_w_in", (j,), qi * 512, 512), [128, KC, 512], wk) for qi in range(2)]
        slots_f = [None, None]
        for h in range(8):
            if h % 4 == 0:
                slots_f[h // 4] = ring_load(wpiece("h_w_in", (j,), 1024 + (h // 4) * 512, 512), [128, KC, 512], wk)
            sq_ = slots_q[h // 4]
            sf_ = slots_f[h // 4]
            nci = h % 4
            bq = bank(next_bank(1, 6))
            for k in range(KC):
                mm(bq, Buf(sq_.ap[:, k, nci * 128:(nci + 1) * 128], sq_.keys), Buf(hnT[:, k, :], [("hnT", k)]),
                   start=(k == 0), stop=(k == KC - 1))
            qb = Buf(acc[:, h % 2, :], [("acc", h % 2)])
            act(qb, bq, AF.Silu)
            bf_ = bank(next_bank(1, 6))
            for k in range(KC):
                mm(bf_, Buf(sf_.ap[:, k, nci * 128:(nci + 1) * 128], sf_.keys), Buf(hnT[:, k, :], [("hnT", k)]),
                   start=(k == 0), stop=(k == KC - 1))
            fb = 3 * (h % 2)
            f0 = Buf(ft4[:, fb + 0, :], K("ft4", fb + 0))
            f1 = Buf(ft4[:, fb + 1, :], K("ft4", fb + 1))
            f2 = Buf(ft4[:, fb + 2, :], K("ft4", fb + 2))
            act(f0, bf_, AF.Sigmoid)
            act(f0, f0, AF.Identity, scale=omlb[:, j, h:h + 1], bias=lbv[:, j, h:h + 1], extra=K("omlb") + K("lbv"))
            tsc(f0, f0, 1e-30, None, ALU.max)
            act(f1, f0, AF.Ln)
            P.op("dve", lambda e, o_=f2.ap, a_=f1.ap, m_=ft4[:, 6, :]: e.tensor_tensor_scan(
                out=o_, data0=m_, data1=a_, initial=0.0, op0=ALU.mult, op1=ALU.add),
                reads=f1.keys + tuple(K("ft4", 6)), writes=f2.keys)
            B3 = f2.ap.rearrange("p (c n) -> p c n", n=CH)
            cp(Buf(Bref[:, h, :], kB), Buf(B3[:, :, CH // 2 - 1], f2.keys), eng="pool")
            cp(Buf(BLv[:, h, :], kBL), Buf(B3[:, :, CH - 1], f2.keys), eng="pool")
            act(f0, f0, AF.Identity, scale=-1.0, bias=onec[:, 0:1], extra=[("onec",)])
            tt(Buf(B3, f2.keys), Buf(B3, f2.keys),
               Buf(Bref[:, h, :].unsqueeze(2).to_broadcast([128, NCH, CH]), kB), ALU.subtract)
            act(f1, f2, AF.Exp)
            tt(Buf(qkT[:, h, :], [("qk", h)]), qb, f1, ALU.mult)
            act(f1, f2, AF.Exp, scale=-1.0)
            tt(Buf(khT[:, h, :], [("kh", h)]), f0, f1, ALU.mult)
        act(Buf(eref, kER), Buf(Bref, kB), AF.Exp)
        act(Buf(eL, kEL), Buf(BLv, kBL), AF.Exp)
        tt(Buf(eLR, kELR), Buf(BLv, kBL), Buf(Bref, kB), ALU.subtract)
        act(Buf(eLR, kELR), Buf(eLR, kELR), AF.Exp)
        for gi_ in range(2):
            slot = ring_load(wpiece("h_w_in", (j,), 3072 + gi_ * 512, 512), [128, KC, 512], wk)

            def consume(nci, b, gi_=gi_):
                c = gi_ * 4 + nci
                act(Buf(gT[:, c, :], [("gT", c)]), b, AF.Silu)
                tsc(Buf(gT[:, c, :], [("gT", c)]), Buf(gT[:, c, :], [("gT", c)]), hnw[:, j, c:c + 1], None, ALU.mult,
                    extra=K("hnw"))

            proj_fm(slot, 512, consume)
        for vi in range(2):
            slot = ring_load(wpiece("h_w_in", (j,), 2048 + vi * 512, 512), [128, KC, 512], wk)
            for s in range(NS):
                b = bank(next_bank(1, 6))
                for k in range(KC):
                    mm(b, Buf(hnT[:, k, s * 128:(s + 1) * 128], [("hnT", k)]), Buf(slot.ap[:, k, :], slot.keys),
                       start=(k == 0), stop=(k == KC - 1))
                P.op("act", lambda e, o=vt[:, s, vi * 4:(vi + 1) * 4, 0:128],
                     i=b.ap.rearrange("p (h n) -> p h n", h=4): e.activation(out=o, in_=i, func=AF.Copy),
                     reads=b.keys, writes=[("vt", s)])

        def mmt(out, lhsT, rhs, start, stop, tp):
            o, l_, r_ = out.ap, lhsT.ap, rhs.ap
            P.op("pe", lambda e: e.matmul(o, l_, r_, start=start, stop=stop, tile_position=tp, skip_group_check=True),
                 reads=lhsT.keys + rhs.keys, writes=out.keys)

        def body(s):
            gi = ti * NS + s
            pb_ = gi % 2
            t0 = s * 128
            ke = Buf(Sm[:, pb_], [("Sm", pb_)])
            tt(Buf(Sm[:, pb_].rearrange("p h (c n) -> p h c n", c=CPS), ke.keys),
               Buf(khT[:, :, t0:t0 + 128].rearrange("p h (c n) -> p h c n", c=CPS), [("kh", h) for h in range(8)]),
               Buf(eLR[:, :, CPS * s:CPS * s + CPS].unsqueeze(3).to_broadcast([128, 8, CPS, CH]), kELR), ALU.mult)
            tpb = Buf(ps[:, 0, :].bitcast(BF16), [("ps", 0)])
            for h in range(8):
                tr(Buf(ps[:, 0, :].bitcast(BF16)[:, h * 128:(h + 1) * 128], [("ps", 0)]),
                   Buf(Sm[:, pb_, h, :], ke.keys), identb)
            kb = Buf(ktm[:, pb_, :], [("ktm", pb_)])
            cp(kb, tpb, eng="act")
            atk = [("ps", 7)]
            for h in range(8):
                for c in range(CPS):
                    r0 = c * CH
                    mmt(Buf(ps[r0:r0 + CH, 7, h * CH:(h + 1) * CH], atk),
                        Buf(khT[:, h, t0 + r0:t0 + r0 + CH], [("kh", h)]),
                        Buf(qkT[:, h, t0 + r0:t0 + r0 + CH], [("qk", h)]), True, True, (0, r0))
            amk = [("amT", pb_)]
            for c in range(CPS):
                r0 = c * CH
                tt(Buf(amT[r0:r0 + CH, pb_, :, r0:r0 + CH], amk),
                   Buf(ps[r0:r0 + CH, 7, 0:8 * CH].rearrange("p (h n) -> p h n", h=8), atk),
                   Buf(cst[r0:r0 + CH, 5, 0:CH].unsqueeze(1).to_broadcast([CH, 8, CH]), K("cst")), ALU.mult)
            sall = Buf(Sst[:, j], K("Sst", j))
            for c in range(CPS):
                gc = CPS * s + c
                r0 = c * CH
                sbk = [("Sbf", c)]
                db = 5 if c % 2 == 0 else 1
                dck = [("ps", db), ("ps", db + 1)]
                tt(Buf(Sbf[:, c], sbk), sall, Buf(eref[:, :, gc:gc + 1].to_broadcast([128, 8, 128]), kER), ALU.mult)
                for h in range(8):
                    mmt(Buf(ps[:, db + h // 4, (h % 4) * 128:(h % 4 + 1) * 128], dck),
                        Buf(ktm[r0:r0 + CH, pb_, h * 128:(h + 1) * 128], kb.keys),
                        Buf(vt[r0:r0 + CH, s, h, 0:128], [("vt", s)]), True, True, (r0, 0))
                tt(sall, sall, Buf(eL[:, :, gc:gc + 1].to_broadcast([128, 8, 128]), kEL), ALU.mult)
                tt(sall, sall, Buf(ps[:, db:db + 2, :].rearrange("p b (q n) -> p (b q) n", q=4), dck), ALU.add)
            yield
            numk = [("ps", 3), ("ps", 4)]
            for h in range(8):
                o = ps[:, 3 + h // 4, (h % 4) * 128:(h % 4 + 1) * 128]
                mmt(Buf(o, numk), Buf(amT[:, pb_, h, :], amk), Buf(vt[:, s, h, 0:128], [("vt", s)]), True, False, None)
                for c in range(CPS):
                    r0 = c * CH
                    mmt(Buf(ps[r0:r0 + CH, 3 + h // 4, (h % 4) * 128:(h % 4 + 1) * 128], numk),
                        Buf(qkT[:, h, t0 + r0:t0 + r0 + CH], [("qk", h)]), Buf(Sbf[:, c, h, :], [("Sbf", c)]),
                        False, True, (0, r0))
            ov = Buf(ps[:, 3:5, :].rearrange("p b (q n) -> p (b q) n", q=4), numk)
            sqb = Buf(sqn.rearrange("p (h n) -> p h n", h=8), [("ft4", 0), ("ft4", 1)])
            r2 = Buf(small[:, 8, 0:8], K("sm", 8))
            act(sqb, ov, AF.Square)
            red(r2, sqb, ALU.add)
            rsqrt_eps(r2, r2, scale=1.0 / 128.0)
            hb = Buf(hh[:, pb_, :], [("hh", pb_)])
            tt(Buf(hh[:, pb_, :].rearrange("p (h n) -> p h n", h=8), hb.keys), ov,
               Buf(r2.ap.unsqueeze(2).to_broadcast([128, 8, 128]), r2.keys), ALU.mult)
            yield
            for c in range(8):
                tr(Buf(ps[:, 0, :].bitcast(BF16)[:, c * 128:(c + 1) * 128], [("ps", 0)]),
                   Buf(hh[:, pb_, c * 128:(c + 1) * 128], hb.keys), identb)
            tt(Buf(hhnT[:, :, t0:t0 + 128], [("hhnT", c) for c in range(8)]),
               Buf(tpb.ap.rearrange("p (c n) -> p c n", c=8), tpb.keys),
               Buf(gT[:, :, t0:t0 + 128], [("gT", c) for c in range(8)]), ALU.mult)
        run_skewed(body)
        out_proj("h_w_out", j, l)

    def ffn_groups(gname, uname, dname, idx, dff, gate_idx=None):
        out = []
        c0 = 0
        while c0 < dff:
            w = min(512, dff - c0)
            out.append((gname, uname, dname, tuple(idx), c0, w, gate_idx))
            c0 += w
        return out

    ffn_ctr = [0]

    def ffn_run(l, groups):
        pend = None

        def down(sd_, hb_i, ncn):
            for dc in range(KC):
                by = bank(5 + dc % 2)
                for nci in range(ncn):
                    mm(by, Buf(sd_.ap[:, nci, dc * 128:(dc + 1) * 128], sd_.keys),
                       Buf(qkT[:, hb_i * 4 + nci, :], [("qk", hb_i * 4 + nci)]), start=(nci == 0), stop=(nci == ncn - 1))
                resid_add(dc, by, gate2(l, dc), l)

        for (gname, uname, dname, idx, c0, w, gate_idx) in groups:
            ncn = w // 128
            sg_ = ring_load(wpiece(gname, idx, c0, w), [128, KC, w], ("wb", gname) + idx)
            su_ = ring_load(wpiece(uname, idx, c0, w), [128, KC, w], ("wb", uname) + idx)
            hb_i = ffn_ctr[0] % 2
            ffn_ctr[0] += 1
            for nci in range(ncn):
                bg = bank(1 + (nci % 2) * 2)
                bu = bank(2 + (nci % 2) * 2)
                for k in range(KC):
                    mm(bg, Buf(sg_.ap[:, k, nci * 128:(nci + 1) * 128], sg_.keys), Buf(hnT[:, k, :], [("hnT", k)]),
                       start=(k == 0), stop=(k == KC - 1))
                for k in range(KC):
                    mm(bu, Buf(su_.ap[:, k, nci * 128:(nci + 1) * 128], su_.keys), Buf(hnT[:, k, :], [("hnT", k)]),
                       start=(k == 0), stop=(k == KC - 1))
                sgb = Buf(ft4[:, nci % 2, :], K("ft4", nci % 2))
                act(sgb, bg, AF.Silu)
                hk = [("qk", hb_i * 4 + nci)]
                if gate_idx is not None:
                    tt(sgb, sgb, Buf(gbc[:, gate_idx, :], [("tmp", gate_idx)]), ALU.mult, eng="pool")
                tt(Buf(qkT[:, hb_i * 4 + nci, :], hk), sgb, bu, ALU.mult)
            if pend is not None:
                down(*pend)
            a_ = wdst[dname]
            for i in idx:
                a_ = a_[i]
            sd_ = ring_load(a_[c0:c0 + w, :].rearrange("(k p) n -> p k n", p=128), [128, ncn, D], ("wb", dname) + idx)
            pend = (sd_, hb_i, ncn)
        if pend is not None:
            down(*pend)

    def moe(l, ti):
        j = l // 2
        lg = Buf(ps[:, 7, 0:NS * 8].rearrange("p (s n) -> p s n", s=NS), [("ps", 7)])
        for s in range(NS):
            o = Buf(ps[:, 7, s * 8:(s + 1) * 8], [("ps", 7)])
            for k in range(KC):
                mm(o, Buf(tmp[:, k, s * 128:(s + 1) * 128], [("tmp", k)]), Buf(rt[:, j, k, :], K("rt")),
                   start=(k == 0), stop=(k == KC - 1))
        L = Buf(small[:, 0, 0:NS * 8].rearrange("p (s n) -> p s n", s=NS), K("sm", 0))
        cp(L, lg)
        m1 = Buf(small[:, 1, 0:NS], K("sm", 1))
        m2 = Buf(small[:, 2, 0:NS], K("sm", 2))
        eq = Buf(small[:, 3, 0:NS * 8].rearrange("p (s n) -> p s n", s=NS), K("sm", 3))
        pe_ = Buf(small[:, 4, 0:NS * 8].rearrange("p (s n) -> p s n", s=NS), K("sm", 4))
        red(m1, L, ALU.max)
        tt(eq, L, Buf(m1.ap.unsqueeze(2).to_broadcast([128, NS, 8]), m1.keys), ALU.is_equal)
        stt(eq, eq, -1e30, L, ALU.mult, ALU.add)
        red(m2, eq, ALU.max)
        tt(eq, L, Buf(m2.ap.unsqueeze(2).to_broadcast([128, NS, 8]), m2.keys), ALU.is_ge)
        tt(pe_, L, Buf(m1.ap.unsqueeze(2).to_broadcast([128, NS, 8]), m1.keys), ALU.subtract)
        act(pe_, pe_, AF.Exp)
        tt(pe_, pe_, eq, ALU.mult)
        red(m2, pe_, ALU.add)
        P.op("dve", lambda e, o=m2.ap, i=m2.ap: e.reciprocal(out=o, in_=i), reads=m2.keys, writes=m2.keys)
        tt(pe_, pe_, Buf(m2.ap.unsqueeze(2).to_broadcast([128, NS, 8]), m2.keys), ALU.mult)
        for e_ in range(NE):
            b = bank(next_bank(1, 6))
            for s in range(NS):
                mm(Buf(b.ap[:, s * 128:(s + 1) * 128], b.keys),
                   Buf(pe_.ap[:, s, e_:e_ + 1].to_broadcast([128, 128]), pe_.keys), ident32)
            P.op("act", lambda e, o=gbc[:, e_, :], i=b.ap: e.activation(out=o, in_=i, func=AF.Copy),
                 reads=b.keys, writes=[("tmp", e_)])
        groups = []
        for e_ in range(NE):
            groups += ffn_groups("moe_w_gate", "moe_w_up", "moe_w_down", (j, e_), D_FFE, gate_idx=e_)
        ffn_run(l, groups)

    for ti in range(NT):
        r0 = ti * TS
        xin = tmp[:].rearrange("p c t -> p (c t)").rearrange("p (s d) -> p s d", s=NS)
        load(Buf(xin, tmpk), x_d[r0:r0 + TS, :].rearrange("(s p) d -> p s d", p=128), "xin")
        for c in range(KC):
            b = bank(next_bank(1, 6))
            for s in range(NS):
                tr(Buf(b.ap[:, s * 128:(s + 1) * 128], b.keys), Buf(xin[:, s, c * 128:(c + 1) * 128], tmpk), ident32)
            P.op("act", lambda e, o=xT[:, c, :], i=b.ap: e.activation(out=o, in_=i, func=AF.Copy),
                 reads=b.keys, writes=[("xT", c)])
        for l in range(NL):
            if DBG <= 1:
                break
            norm_mod(l, 1, False)
            if DBG <= 2:
                break
            if l % 2 == 0:
                mlstm(l, ti)
            else:
                hgrn(l, ti)
            if half == "mixer" and l == NL - 1:
                break
            if l % 2 == 0:
                norm_mod(l, 2, False)
                ffn_run(l, ffn_groups("ffn_w_gate", "ffn_w_up", "ffn_w_down", (l // 2,), D_FF))
            else:
                norm_mod(l, 2, True)
                moe(l, ti)
        if final:
            rms_stats()
            tt(Buf(tmp[:], tmpk), Buf(xT[:], xk), Buf(rstd[:].unsqueeze(1).to_broadcast([128, KC, TS]), K("rstd")),
               ALU.mult)
            for c in range(KC):
                tsc(Buf(tmp[:, c, :], [("tmp", c)]), Buf(tmp[:, c, :], [("tmp", c)]), fnw[:, c:c + 1], None, ALU.mult,
                    extra=K("fnw"))
            src, srck, stg, stgk = tmp, "tmp", xT, xk
        else:
            src, srck, stg, stgk = xT, "xT", tmp, tmpk
        yout = stg[:].rearrange("p c t -> p (c t)").rearrange("p (s d) -> p s d", s=NS)
        for s in range(NS):
            for half in range(2):
                b = bank(next_bank(1, 6))
                for cc in range(4):
                    c = half * 4 + cc
                    tr(Buf(b.ap[:, cc * 128:(cc + 1) * 128], b.keys), Buf(src[:, c, s * 128:(s + 1) * 128], [(srck, c)]),
                       ident32)
                P.op("act", lambda e, o=yout[:, s, half * 512:(half + 1) * 512], i=b.ap: e.activation(
                    out=o, in_=i, func=AF.Copy), reads=b.keys, writes=stgk)
        P.dma("sp", lambda e, o=out_d[r0:r0 + TS, :].rearrange("(s p) d -> p s d", p=128), i=yout: e.dma_start(
            out=o, in_=i), "out", reads=stgk)

    fch = ["out"]
    if os.environ.get("KDUMP"):
        dbg_d = nc.dram_tensor("dbg", [128, 20 * 64], F32, kind="ExternalOutput").ap()
        P.dma("sp", lambda e: e.dma_start(out=dbg_d, in_=small[:].rearrange("p a n -> p (a n)")), "dbgc",
              reads=[("sm", i) for i in range(20)])
        fch.append("dbgc")
    P.emit(nc, fch)
    st.close()
    return nc


def _fm(v):
    v = np.asarray(v, np.float32)
    lead = v.shape[:-1]
    a = v.reshape(lead + (KC, 128))
    a = np.moveaxis(a, -1, 0)
    return np.ascontiguousarray(a)


def _consts():
    c = np.zeros((128, 6, 128), np.float32)
    i = np.arange(128)
    c[:, 0, :] = np.eye(128, dtype=np.float32)
    tri = (i[:, None] <= i[None, :]).astype(np.float32)
    c[:, 1, :] = tri
    c[:, 2, :] = -tri
    c[:, 3, :] = 1.0
    c[:, 4, :] = 1.0 / 1024.0
    c[:, 5, 0:32] = ((i[:, None] % 32) <= np.arange(32)[None, :]).astype(np.float32)
    return c


def make_in_maps(inputs, NT, cores):
    I = {k: np.asarray(v) for k, v in inputs.items()}
    shared = {
        "ada_w": np.ascontiguousarray(I["ada_w"], np.float32),
        "ada_b": np.ascontiguousarray(np.moveaxis(I["ada_b"].reshape(DEPTH, 48, 128), -1, 0), np.float32),
        "norm1_w": _fm(I["norm1_w"]),
        "norm2_w": _fm(I["norm2_w"]),
        "final_norm_w": _fm(I["final_norm_w"]),
        "m_gate_bias": np.ascontiguousarray(np.broadcast_to(
            np.concatenate([I["m_i_bias"], I["m_f_bias"]], axis=-1)[None], (128, 2, 16)), np.float32),
        "m_conv_w": np.ascontiguousarray(np.transpose(_fm(I["m_conv_w"]), (0, 1, 3, 2)), np.float32),
        "m_conv_b": _fm(I["m_conv_b"]),
        "m_norm_w": _fm(I["m_norm_w"]),
        "h_norm_w": _fm(I["h_norm_w"]),
        "h_lb_logits": _fm(I["h_lb_logits"]),
        "moe_router": np.ascontiguousarray(
            np.transpose(I["moe_router"].reshape(2, KC, 128, NE), (2, 0, 1, 3)), np.float32),
        "consts": _consts(),
    }
    for n in ("m_w_in", "m_w_out", "h_w_in", "h_w_out", "ffn_w_gate", "ffn_w_up", "ffn_w_down",
              "moe_w_gate", "moe_w_up", "moe_w_down"):
        shared[n] = np.ascontiguousarray(I[n], np.float32)
    maps = []
    for b in cores:
        m = dict(shared)
        m["x"] = np.ascontiguousarray(I["x"][b, :NT * TS, :], np.float32)
        m["c"] = _fm(I["c"][b])
        maps.append(m)
    return maps


_NC_CACHE = {}


def run(inputs, NT=SEQ // TS, NL=DEPTH, final=True, cores=tuple(range(8)), trace=False, half="full"):
    key = (NT, NL, final, half)
    if key not in _NC_CACHE:
        _NC_CACHE[key] = build(NT, NL, final, half)
    nc = _NC_CACHE[key]
    maps = make_in_maps(inputs, NT, cores)
    res = run_bass_kernel_spmd(nc, maps, core_ids=list(range(len(cores))), trace=trace)
    out = np.stack([np.asarray(r["out"], np.float32) for r in res.results], axis=0)
    if os.environ.get("KDUMP"):
        np.save(os.environ["KDUMP"], np.asarray(res.results[0]["dbg"]))
    return out, res


def kernel(**inputs):
    out, _ = run(inputs)
    return out.astype(np.float32)
```
